# Optimizing a Trainium2 kernel written in Bass

```python
import jax, jax.numpy as jnp
from jax import lax
import numpy as np

D_MODEL = 1024
BATCH = 16
SEQ = 4096
DEPTH = 1

MEM_LEN = 256
HEAD_DIM = 64
CONV_CH = D_MODEL // 4
ATTN_HEADS = (3 * D_MODEL // 4) // HEAD_DIM
ATTN_WIDTH = ATTN_HEADS * HEAD_DIM
MIX_WIDTH = CONV_CH + ATTN_WIDTH
IN_WIDTH = 2 * CONV_CH + 3 * ATTN_WIDTH
CONV_KERNEL = 31
DILATED_BRANCHES = ((128, 1), (512, 4), (2048, 16))
ROPE_THETA = 10000.0
MEM_HEADS = 4
MEM_HEAD_DIM = D_MODEL // MEM_HEADS
N_GROUPS = 4
EXPERTS_PER_GROUP = 8
N_EXPERTS = N_GROUPS * EXPERTS_PER_GROUP
TOP_K = 2
EXPERT_FF = D_MODEL // 2
DISPATCH_BLOCK = 256
NORM_EPS = 1e-6
LN_EPS = 1e-5

kernel_name = 'hybrid_conv_dilated_attn_hmoe_encoder'


def rms_norm(t, g):
    tf = t.astype(jnp.float32)
    y = tf * lax.rsqrt(jnp.mean(tf * tf, axis=-1, keepdims=True) + NORM_EPS)
    return (y * g.astype(jnp.float32)).astype(t.dtype)


def layer_norm(t, g, b):
    tf = t.astype(jnp.float32)
    mu = jnp.mean(tf, axis=-1, keepdims=True)
    var = jnp.mean(jnp.square(tf - mu), axis=-1, keepdims=True)
    y = (tf - mu) * lax.rsqrt(var + LN_EPS)
    return (y * g.astype(jnp.float32) + b.astype(jnp.float32)).astype(t.dtype)


def rotary(t, positions):
    half = t.shape[-1] // 2
    inv = 1.0 / (ROPE_THETA ** (jnp.arange(half, dtype=jnp.float32) * (2.0 / t.shape[-1])))
    ang = positions.astype(jnp.float32)[:, None] * inv[None, :]
    cos, sin = jnp.cos(ang), jnp.sin(ang)
    t1, t2 = t[..., :half], t[..., half:]
    return jnp.concatenate([t1 * cos - t2 * sin, t1 * sin + t2 * cos], axis=-1)


def conformer_conv(conv_in, dw_w, dw_b, ln_g, ln_b):
    a, gate = conv_in[..., :CONV_CH], conv_in[..., CONV_CH:]
    c = a * jax.nn.sigmoid(gate)
    pad = CONV_KERNEL // 2
    c = lax.conv_general_dilated(
        c, dw_w.reshape(CONV_KERNEL, 1, CONV_CH).astype(c.dtype),
        window_strides=(1,), padding=[(pad, pad)],
        dimension_numbers=('NWC', 'WIO', 'NWC'),
        feature_group_count=CONV_CH) + dw_b.astype(c.dtype)
    c = layer_norm(c, ln_g, ln_b)
    return jax.nn.silu(c)


def dilated_branch(q, k, v, window, dilation):
    B_, H, S_, Dh = q.shape
    half = window // (2 * dilation)
    blk = half
    L = S_ // dilation
    nb = -(-L // blk)
    Lp = nb * blk
    pad = Lp - L

    def to_sub(t):
        return t.reshape(B_, H, L, dilation, Dh).transpose(0, 1, 3, 2, 4)

    qs, ks, vs = to_sub(q), to_sub(k), to_sub(v)
    qs = jnp.pad(qs, ((0, 0), (0, 0), (0, 0), (0, pad), (0, 0)))
    kp = jnp.pad(ks, ((0, 0), (0, 0), (0, 0), (blk, pad + blk), (0, 0)))
    vp = jnp.pad(vs, ((0, 0), (0, 0), (0, 0), (blk, pad + blk), (0, 0)))
    qb = qs.reshape(B_, H, dilation, nb, blk, Dh)
    kb = kp.reshape(B_, H, dilation, nb + 2, blk, Dh)
    vb = vp.reshape(B_, H, dilation, nb + 2, blk, Dh)
    kw = jnp.concatenate([kb[:, :, :, :-2], kb[:, :, :, 1:-1], kb[:, :, :, 2:]], axis=-2)
    vw = jnp.concatenate([vb[:, :, :, :-2], vb[:, :, :, 1:-1], vb[:, :, :, 2:]], axis=-2)

    qi = np.arange(nb)[:, None] * blk + np.arange(blk)[None, :]
    ki = np.arange(nb)[:, None] * blk - blk + np.arange(3 * blk)[None, :]
    valid = ((np.abs(qi[:, :, None] - ki[:, None, :]) <= half)
             & (ki[:, None, :] >= 0) & (ki[:, None, :] < L))

    s = jnp.einsum('bhrnqd,bhrnkd->bhrnqk', qb, kw) * (HEAD_DIM ** -0.5)
    s = jnp.where(jnp.asarray(valid), s, -jnp.inf)
    m = jnp.max(s, axis=-1)
    p = jnp.exp(s - m[..., None])
    den = jnp.sum(p, axis=-1)
    num = jnp.einsum('bhrnqk,bhrnkd->bhrnqd', p, vw)

    def back(t):
        tail = tuple(t.shape[5:])
        t = t.reshape((B_, H, dilation, Lp) + tail)[:, :, :, :L]
        perm = (0, 1, 3, 2) + tuple(range(4, 4 + len(tail)))
        return t.transpose(perm).reshape((B_, H, S_) + tail)

    return back(num), back(m), back(den)


def dilated_attention(q, k, v, positions):
    B_, H, S_, Dh = q.shape
    out_dtype = q.dtype
    qf = rotary(q.astype(jnp.float32), positions)
    kf = rotary(k.astype(jnp.float32), positions)
    vf = v.astype(jnp.float32)
    num_acc = den_acc = m_acc = None
    for window, dilation in DILATED_BRANCHES:
        num, m, den = dilated_branch(qf, kf, vf, window, dilation)
        if m_acc is None:
            num_acc, den_acc, m_acc = num, den, m
        else:
            m_new = jnp.maximum(m_acc, m)
            a = jnp.exp(m_acc - m_new)
            b = jnp.exp(m - m_new)
            num_acc = num_acc * a[..., None] + num * b[..., None]
            den_acc = den_acc * a + den * b
            m_acc = m_new
    o = num_acc / den_acc[..., None]
    return o.transpose(0, 2, 1, 3).reshape(B_, S_, H * Dh).astype(out_dtype)


def memory_cross_attention(h, mem_n, w_q, w_k, w_v, w_o):
    B_, S_, D = h.shape
    q = (h @ w_q).reshape(B_, S_, MEM_HEADS, MEM_HEAD_DIM)
    k = (mem_n @ w_k).reshape(B_, -1, MEM_HEADS, MEM_HEAD_DIM)
    v = (mem_n @ w_v).reshape(B_, -1, MEM_HEADS, MEM_HEAD_DIM)
    s = jnp.einsum('bshd,bmhd->bhsm', q, k, preferred_element_type=jnp.float32) * (MEM_HEAD_DIM ** -0.5)
    p = jax.nn.softmax(s, axis=-1).astype(v.dtype)
    o = jnp.einsum('bhsm,bmhd->bshd', p, v).reshape(B_, S_, D)
    return o @ w_o


def hierarchical_moe(h, w_group, b_group, w_router, b_router, w1, w3, w2):
    B_, S_, D = h.shape
    T = B_ * S_
    hf = h.reshape(T, D)
    gl = (hf @ w_group + b_group).astype(jnp.float32)
    gp = jax.nn.softmax(gl, axis=-1)
    _, g_sel = lax.top_k(gl, 1)
    g_idx = g_sel[:, 0]
    tok_idx = jnp.arange(T, dtype=jnp.int32)
    p_g = gp[tok_idx, g_idx]
    el = (hf @ w_router + b_router).astype(jnp.float32).reshape(T, N_GROUPS, EXPERTS_PER_GROUP)
    el_sel = el[tok_idx, g_idx]
    top_v, top_i = lax.top_k(el_sel, TOP_K)
    gates = p_g[:, None] * jax.nn.softmax(top_v, axis=-1)
    expert_ids = g_idx[:, None] * EXPERTS_PER_GROUP + top_i

    A = T * TOP_K
    flat_e = expert_ids.reshape(A).astype(jnp.int32)
    flat_t = jnp.repeat(tok_idx, TOP_K)
    flat_g = gates.reshape(A)
    order = jnp.argsort(flat_e)
    se, st, sg = flat_e[order], flat_t[order], flat_g[order]
    counts = jnp.bincount(flat_e, length=N_EXPERTS).astype(jnp.int32)
    starts = jnp.cumsum(counts) - counts
    pcounts = (counts + DISPATCH_BLOCK - 1) // DISPATCH_BLOCK * DISPATCH_BLOCK
    pstarts = jnp.cumsum(pcounts) - pcounts
    dest = pstarts[se] + jnp.arange(A, dtype=jnp.int32) - starts[se]
    P = (-(-A // DISPATCH_BLOCK) + N_EXPERTS) * DISPATCH_BLOCK
    n_blk = P // DISPATCH_BLOCK
    row_tok = jnp.full((P,), T, dtype=jnp.int32).at[dest].set(st)
    row_gate = jnp.zeros((P,), jnp.float32).at[dest].set(sg)
    blk_e = jnp.minimum(
        jnp.searchsorted(pstarts + pcounts, jnp.arange(n_blk, dtype=jnp.int32) * DISPATCH_BLOCK,
                         side='right'),
        N_EXPERTS - 1).astype(jnp.int32)
    x_rows = jnp.concatenate([hf, jnp.zeros((1, D), hf.dtype)], axis=0)[row_tok]
    x_blocks = x_rows.reshape(n_blk, DISPATCH_BLOCK, D)

    def expert_block(args):
        xb, e = args
        return (jax.nn.silu(xb @ w1[e]) * (xb @ w3[e])) @ w2[e]

    y_rows = lax.map(expert_block, (x_blocks, blk_e)).reshape(P, D)
    y = jax.ops.segment_sum(y_rows * row_gate[:, None].astype(y_rows.dtype), row_tok,
                            num_segments=T + 1)[:T]
    return y.reshape(B_, S_, D)


def setup_inputs(seed: int = 0) -> dict:
    key = jax.random.key(seed)
    ks = jax.random.split(key, 26)
    f32 = jnp.float32
    D = D_MODEL

    def nrm(k, shape, scale):
        return jax.random.normal(k, shape, f32) * scale

    def gain(k, shape):
        return 1.0 + 0.02 * jax.random.normal(k, shape, f32)

    return {
        'x': jax.random.normal(ks[0], (BATCH, SEQ, D), f32),
        'mem': jax.random.normal(ks[1], (BATCH, MEM_LEN, D), f32),
        'positions': jnp.arange(SEQ, dtype=jnp.int32),
        'mix_norm_g': gain(ks[2], (DEPTH, D)),
        'w_in': nrm(ks[3], (DEPTH, D, IN_WIDTH), D ** -0.5),
        'conv_dw_w': nrm(ks[4], (DEPTH, CONV_KERNEL, CONV_CH), CONV_KERNEL ** -0.5),
        'conv_dw_b': nrm(ks[5], (DEPTH, CONV_CH), 0.02),
        'conv_ln_g': gain(ks[6], (DEPTH, CONV_CH)),
        'conv_ln_b': nrm(ks[7], (DEPTH, CONV_CH), 0.02),
        'conv_out_g': gain(ks[8], (DEPTH, CONV_CH)),
        'attn_out_g': gain(ks[9], (DEPTH, ATTN_WIDTH)),
        'w_out': nrm(ks[10], (DEPTH, MIX_WIDTH, D), MIX_WIDTH ** -0.5),
        'xattn_norm_g': gain(ks[11], (DEPTH, D)),
        'mem_norm_g': gain(ks[12], (DEPTH, D)),
        'w_xq': nrm(ks[13], (DEPTH, D, D), D ** -0.5),
        'w_xk': nrm(ks[14], (DEPTH, D, D), D ** -0.5),
        'w_xv': nrm(ks[15], (DEPTH, D, D), D ** -0.5),
        'w_xo': nrm(ks[16], (DEPTH, D, D), D ** -0.5),
        'moe_norm_g': gain(ks[17], (DEPTH, D)),
        'w_group': nrm(ks[18], (DEPTH, D, N_GROUPS), D ** -0.5),
        'b_group': nrm(ks[19], (DEPTH, N_GROUPS), 0.01),
        'w_router': nrm(ks[20], (DEPTH, D, N_EXPERTS), D ** -0.5),
        'b_router': nrm(ks[21], (DEPTH, N_EXPERTS), 0.01),
        'w1': nrm(ks[22], (DEPTH, N_EXPERTS, D, EXPERT_FF), D ** -0.5),
        'w3': nrm(ks[23], (DEPTH, N_EXPERTS, D, EXPERT_FF), D ** -0.5),
        'w2': nrm(ks[24], (DEPTH, N_EXPERTS, EXPERT_FF, D), EXPERT_FF ** -0.5),
        'final_norm_g': gain(ks[25], (D,)),
    }


def reference(x, mem, positions, mix_norm_g, w_in, conv_dw_w, conv_dw_b, conv_ln_g, conv_ln_b,
              conv_out_g, attn_out_g, w_out, xattn_norm_g, mem_norm_g, w_xq, w_xk, w_xv, w_xo,
              moe_norm_g, w_group, b_group, w_router, b_router, w1, w3, w2, final_norm_g):
    B_, S_, _ = x.shape
    for l in range(DEPTH):
        h = rms_norm(x, mix_norm_g[l])
        u = h @ w_in[l]
        conv_in = u[..., :2 * CONV_CH]
        qkv = u[..., 2 * CONV_CH:].reshape(B_, S_, 3, ATTN_HEADS, HEAD_DIM).transpose(2, 0, 3, 1, 4)
        c = conformer_conv(conv_in, conv_dw_w[l], conv_dw_b[l], conv_ln_g[l], conv_ln_b[l])
        a = dilated_attention(qkv[0], qkv[1], qkv[2], positions)
        mixed = jnp.concatenate([rms_norm(c, conv_out_g[l]), rms_norm(a, attn_out_g[l])], axis=-1)
        x = x + mixed @ w_out[l]
        h = rms_norm(x, xattn_norm_g[l])
        m = rms_norm(mem, mem_norm_g[l])
        x = x + memory_cross_attention(h, m, w_xq[l], w_xk[l], w_xv[l], w_xo[l])
        h = rms_norm(x, moe_norm_g[l])
        x = x + hierarchical_moe(h, w_group[l], b_group[l], w_router[l], b_router[l], w1[l], w3[l], w2[l])
    return rms_norm(x, final_norm_g)
```

```python
import numpy as np
import ml_dtypes
import concourse.bass as bass
import concourse.mybir as mybir
from concourse.bass_utils import run_bass_kernel_spmd

F32 = mybir.dt.float32
BF16 = mybir.dt.bfloat16
I32 = mybir.dt.int32
AF = mybir.ActivationFunctionType
ALU = mybir.AluOpType
AX = mybir.AxisListType

S = 4096
D = 1024
NT = S // 128
NCH = S // 512
MEM = 256
NEXP = 32
CAP = 1024
NSLOT = NEXP * CAP
EPS = 1e-6
LN_EPS = 1e-5
SEM_LIMIT = 30000


class Buf:
    __slots__ = ("name", "w", "r", "excl")

    def __init__(self, name, excl=False):
        self.name = name
        self.w = None
        self.r = {}
        self.excl = excl


class K:
    def __init__(self, nc):
        self.nc = nc
        self.eng = {"pe": nc.tensor, "act": nc.scalar, "dve": nc.vector, "pool": nc.gpsimd, "sp": nc.sync}
        self.sem = {}
        self.cnt = {}
        self.own = {e: set() for e in self.eng}
        self.seen = {e: {} for e in self.eng}
        self.nsem = 0
        self.allsems = []
        for e in self.eng:
            self._newsem(e)
        self.dq = {}
        for q, n in (("sp", 12), ("pool", 12), ("poolz", 6)):
            sems = [self._mksem("d_%s%d" % (q, i)) for i in range(n)]
            self.dq[q] = {"sems": sems, "cnt": [0] * len(sems), "rr": 0}
        self.det_ev = {}
        self.pending = {e: False for e in self.eng}
        self.last_ev = {}

    def _mksem(self, name):
        s = self.nc.alloc_semaphore(name)
        self.nsem += 1
        self.allsems.append(s)
        return s

    def _newsem(self, e):
        self.sem[e] = self._mksem("s_%s%d" % (e, self.nsem))
        self.own[e].add(self.sem[e])
        self.cnt[e] = 0

    def wait(self, e, evs):
        need = {}
        for ev in evs:
            if ev is None:
                continue
            sem, val = ev
            if e == "pe" and sem is self.sem["pe"]:
                continue
            if sem is self.sem[e] and val > self.cnt[e]:
                continue
            if self.seen[e].get(sem, 0) < val and need.get(sem, 0) < val:
                need[sem] = val
        for sem, val in need.items():
            self.eng[e].wait_ge(sem, val)
            self.seen[e][sem] = val

    def _deps(self, reads, writes, e=None):
        evs = []
        for b in reads:
            evs.append(b.w)
            if b.excl:
                evs.extend(it for it in b.r.items() if it[0] not in self.own.get(e, ()))
        for b in writes:
            evs.append(b.w)
            evs.extend(b.r.items())
        return evs

    def _mark(self, ev, reads, writes):
        sem, val = ev
        for b in reads:
            if b.r.get(sem, 0) < val:
                b.r[sem] = val
        for b in writes:
            b.w = ev
            b.r = {}
        self.last_ev[sem] = max(self.last_ev.get(sem, 0), val)

    def op(self, e, fn, reads=(), writes=(), inc=True):
        self.wait(e, self._deps(reads, writes, e))
        ins = fn(self.eng[e])
        if inc:
            if self.cnt[e] >= SEM_LIMIT and not self.pending[e]:
                self._newsem(e)
            self.cnt[e] += 1
            ins.then_inc(self.sem[e], 1)
            ev = (self.sem[e], self.cnt[e])
            self.pending[e] = False
        else:
            ev = (self.sem[e], self.cnt[e] + 1)
            self.pending[e] = True
        self._mark(ev, reads, writes)
        return ev

    def dma(self, q, fn, reads=(), writes=(), pre_nop=False):
        dq = self.dq[q]
        i = dq["rr"]
        dq["rr"] = (i + 1) % len(dq["sems"])
        sem = dq["sems"][i]
        evs = self._deps(reads, writes)
        evs.append((sem, 16 * dq["cnt"][i]) if dq["cnt"][i] else None)
        qe = "pool" if q == "poolz" else q
        self.wait(qe, evs)
        if pre_nop:
            self.eng[qe].nop()
        ins = fn(self.eng[qe])
        dq["cnt"][i] += 1
        ins.then_inc(sem, 16)
        ev = (sem, 16 * dq["cnt"][i])
        self._mark(ev, reads, writes)
        if q == "poolz":
            self.det_ev[sem] = ev[1]
            if self.last_ev.get(sem) == ev[1]:
                del self.last_ev[sem]
        return ev

    def join_detached(self):
        for sem, val in self.det_ev.items():
            self.last_ev[sem] = max(self.last_ev.get(sem, 0), val)
        self.det_ev = {}

    def pe_cond(self, reg, thresh, n_inc, body):
        body()

    def barrier(self):
        evs = list(self.last_ev.items())
        for e in self.eng:
            self.wait(e, evs)

    def finish(self, e="sp"):
        self.wait(e, list(self.last_ev.items()))


def _bf(a):
    return np.ascontiguousarray(a.astype(np.float32))


def host_consts():
    c = {}
    c["c_ident"] = np.eye(128, dtype=np.float32)
    rt = np.zeros((128, 128), np.float32)
    for h in range(2):
        for i in range(32):
            rt[h * 64 + i + 32, h * 64 + i] = -1.0
            rt[h * 64 + i, h * 64 + i + 32] = 1.0
    c["c_rot"] = rt
    jj = np.arange(128)[:, None]
    ii = np.arange(128)[None, :]
    mlo = (ii >= jj).astype(np.float32)
    mhi = (ii <= jj).astype(np.float32)
    c["c_masklh"] = np.concatenate([mlo, mhi], axis=1)
    c["c_maskf"] = np.concatenate([mhi[64:128], np.zeros((64, 128), np.float32)], axis=0)
    c["c_tri"] = (jj < ii).astype(np.float32)
    half = 32
    inv = (1.0 / (np.float32(10000.0) ** (np.arange(half, dtype=np.float32) * np.float32(2.0 / 64)))).astype(np.float32)
    c["c_inv"] = np.tile(inv, 4).reshape(128, 1).astype(np.float32)
    c["c_ec"] = np.tile((np.arange(NEXP, dtype=np.float32) * CAP)[None, :], (128, 1))
    es = np.zeros((128, 64), np.float32)
    es[64, :] = 1.0
    c["c_esel"] = es
    return c


CONST_SHAPES = {"c_ident": [128, 128], "c_rot": [128, 128], "c_masklh": [128, 256], "c_maskf": [128, 128],
                "c_tri": [128, 128], "c_inv": [128, 1], "c_ec": [128, NEXP], "c_esel": [128, 64]}

WEIGHT_SHAPES = {
    "mix_norm_g": [D], "w_in": [D, 2816], "conv_dw_w": [31, 256], "conv_dw_b": [256], "conv_ln_g": [256],
    "conv_ln_b": [256], "conv_out_g": [256], "attn_out_g": [768], "w_out": [D, D], "xattn_norm_g": [D],
    "mem_norm_g": [D], "w_xq": [D, D], "w_xk": [D, D], "w_xv": [D, D], "w_xo": [D, D], "moe_norm_g": [D],
    "w_group": [D, 4], "b_group": [4], "w_router": [D, 32], "b_router": [32],
    "w1": [NEXP, D, 512], "w3": [NEXP, D, 512], "w2": [NEXP, 512, D], "final_norm_g": [D],
}


from contextlib import ExitStack

PI_SAFE = 3.141592
TWO_PI = 6.283185307179586
C1 = 6.28125
C2 = TWO_PI - C1


def build(nseq=2, stage=99, dbg=False, lim=None):
    lim = lim or {}
    nc = bass.Bass("TRN2", target_bir_lowering=False)
    T = nseq * S
    din = {}
    din["x"] = nc.dram_tensor("x", [T, D], F32, kind="ExternalInput")
    din["mem"] = nc.dram_tensor("mem", [nseq * MEM, D], F32, kind="ExternalInput")
    din["positions"] = nc.dram_tensor("positions", [S], I32, kind="ExternalInput")
    for k_, shp in WEIGHT_SHAPES.items():
        din[k_] = nc.dram_tensor(k_, shp, F32, kind="ExternalInput")
    for k_, shp in CONST_SHAPES.items():
        din[k_] = nc.dram_tensor(k_, shp, F32, kind="ExternalInput")
    out_d = nc.dram_tensor("out", [T, D], F32, kind="ExternalOutput")
    kscr = "ExternalOutput" if dbg else "Internal"
    o_scr = nc.dram_tensor("o_scr", [nseq, 768, S], BF16, kind=kscr)
    x2_scr = nc.dram_tensor("x2_scr", [T, D], F32, kind=kscr)
    tab_scr = nc.dram_tensor("tab_scr", [2, 128, S], BF16)
    h3_scr = nc.dram_tensor("h3_scr", [T, D], BF16)
    xs_scr = nc.dram_tensor("xs_scr", [NSLOT + 128, D], BF16)
    ys_scr = nc.dram_tensor("ys_scr", [NSLOT + 128, D], BF16)
    dbg_d = {}
    if dbg:
        dbg_d["d_hT"] = nc.dram_tensor("d_hT", [nseq, 128, 8, S], BF16, kind="ExternalOutput")
        dbg_d["d_zT"] = nc.dram_tensor("d_zT", [nseq, 128, 2, S], BF16, kind="ExternalOutput")
        dbg_d["d_rt"] = nc.dram_tensor("d_rt", [128, nseq * NT * 4], F32, kind="ExternalOutput")

    k = K(nc)
    A = nc.alloc_sbuf_tensor

    def pe(fn, r=(), w=(), inc=True):
        return k.op("pe", fn, r, w, inc)

    def act(fn, r=(), w=()):
        return k.op("act", fn, r, w)

    def dve(fn, r=(), w=()):
        return k.op("dve", fn, r, w)

    def pool(fn, r=(), w=()):
        return k.op("pool", fn, r, w)

    uid = [0]

    def salloc(stack, name, shape, dtype):
        uid[0] += 1
        return stack.enter_context(nc.sbuf_tensor("%s_u%d" % (name, uid[0]), shape, dtype))

    ps2 = [nc.alloc_psum_tensor("ps2_%d" % i, [128, 1024], F32) for i in range(4)]
    ps = []
    for i in range(4):
        ps.append(ps2[i][:, 0:512])
        ps.append(ps2[i][:, 512:1024])
    psbf = [p.bitcast(BF16) for p in ps]
    psb = [Buf("ps%d" % i, excl=True) for i in range(8)]

    def load_const(name, src, shape, dtype):
        t = A(name, shape, dtype)
        b = Buf(name)
        q = "sp" if dtype == F32 else "pool"
        k.dma(q, lambda e: e.dma_start(out=t[:], in_=src), writes=[b])
        return t, b

    ident_f, ident_f_b = load_const("ident_f", din["c_ident"].ap(), [128, 128], F32)
    ident_b, ident_b_b = load_const("ident_b", din["c_ident"].ap(), [128, 128], BF16)
    rot_b, rot_b_b = load_const("rot_b", din["c_rot"].ap(), [128, 128], BF16)
    masklh, masklh_b = load_const("masklh", din["c_masklh"].ap(), [128, 256], BF16)
    maskf, maskf_b = load_const("maskf", din["c_maskf"].ap(), [128, 128], BF16)
    tri_b, tri_b_b = load_const("tri_b", din["c_tri"].ap(), [128, 128], BF16)
    inv_t, inv_b = load_const("inv_t", din["c_inv"].ap(), [128, 1], F32)
    ec_t, ec_b = load_const("ec_t", din["c_ec"].ap(), [128, NEXP], F32)
    esel, esel_b = load_const("esel", din["c_esel"].ap(), [128, 64], F32)

    ones_b = A("ones_b", [128, 128], BF16)
    ones_b_b = Buf("ones_b")
    dve(lambda e: e.memset(ones_b[:], 1.0), w=[ones_b_b])
    ones_f = A("ones_f", [128, 128], F32)
    ones_f_b = Buf("ones_f")
    dve(lambda e: e.memset(ones_f[:], 1.0), w=[ones_f_b])

    def gvec(name, src_ap, nchunk):
        t = A(name, [128, nchunk], F32)
        b = Buf(name)
        with nc.allow_non_contiguous_dma(reason="tiny gain vector"):
            k.dma("sp", lambda e: e.dma_start(out=t[:], in_=src_ap.rearrange("(c p) -> p c", p=128)), writes=[b])
        return t, b

    g_mix, g_mix_b = gvec("g_mix", din["mix_norm_g"].ap(), 8)
    g_xn, g_xn_b = gvec("g_xn", din["xattn_norm_g"].ap(), 8)
    g_mem, g_mem_b = gvec("g_mem", din["mem_norm_g"].ap(), 8)
    g_mx = A("g_mx", [128, 8], F32)
    g_mx_b = Buf("g_mx")
    with nc.allow_non_contiguous_dma(reason="tiny gain vector"):
        k.dma("sp", lambda e: e.dma_start(out=g_mx[:, 0:2], in_=din["conv_out_g"].ap().rearrange("(c p) -> p c", p=128)), writes=[g_mx_b])
        k.dma("sp", lambda e: e.dma_start(out=g_mx[:, 2:8], in_=din["attn_out_g"].ap().rearrange("(c p) -> p c", p=128)), writes=[g_mx_b])
    cv_b, cv_b_b = gvec("cv_b", din["conv_dw_b"].ap(), 2)
    ln_g, ln_g_b = gvec("ln_g", din["conv_ln_g"].ap(), 2)
    ln_b, ln_b_b = gvec("ln_b", din["conv_ln_b"].ap(), 2)
    wdw = A("wdw", [128, 2, 31], F32)
    wdw_b = Buf("wdw")
    with nc.allow_non_contiguous_dma(reason="tiny conv weights"):
        for c2 in range(2):
            k.dma("sp", lambda e: e.dma_start(out=wdw[:, c2, :], in_=din["conv_dw_w"].ap()[:, c2 * 128:(c2 + 1) * 128].rearrange("k p -> p k")), writes=[wdw_b])

    def bcast_rows(name, src_ap, n, dst=None, off=0):
        if dst is None:
            t = A(name, [128, n], F32)
            b = Buf(name)
            k.dma("sp", lambda e: e.dma_start(out=t[:], in_=src_ap.partition_broadcast(128)), writes=[b])
            return t, b
        k.dma("sp", lambda e: e.dma_start(out=dst[0][:, off:off + n], in_=src_ap.partition_broadcast(128)), writes=[dst[1]])

    gfin, gfin_b = bcast_rows("gfin", din["final_norm_g"].ap(), D)
    gmoe, gmoe_b = bcast_rows("gmoe", din["moe_norm_g"].ap(), D)
    brt = A("brt", [128, 36], F32)
    brt_b = Buf("brt")
    bcast_rows(None, din["b_group"].ap(), 4, dst=(brt, brt_b), off=0)
    bcast_rows(None, din["b_router"].ap(), 32, dst=(brt, brt_b), off=4)
    wr = A("wr", [128, 8, 36], F32)
    wr_b = Buf("wr")
    with nc.allow_non_contiguous_dma(reason="small router weights"):
        k.dma("sp", lambda e: e.dma_start(out=wr[:, :, 0:4], in_=din["w_group"].ap().rearrange("(c p) n -> p c n", p=128)), writes=[wr_b])
        k.dma("sp", lambda e: e.dma_start(out=wr[:, :, 4:36], in_=din["w_router"].ap().rearrange("(c p) n -> p c n", p=128)), writes=[wr_b])

    wrb = A("wrb", [128, 8, 36], BF16)
    wrb_b = Buf("wrb")
    k.barrier()
    dve(lambda e: e.tensor_copy(out=wrb[:], in_=wr[:]), r=[wr_b], w=[wrb_b])
    NTT = nseq * NT
    slot_i = A("slot_i", [128, NTT * 2], I32)
    gate_f = A("gate_f", [128, NTT * 2], F32)
    rt_b = Buf("rt")
    base_t = A("base_t", [128, NEXP], F32)
    base_b = Buf("base")
    dve(lambda e: e.memset(base_t[:], 0.0), w=[base_b])
    zT = A("zT", [128, 2, S], BF16)
    zT_b = [Buf("zT%d" % i) for i in range(NCH)]

    zt = A("zt", [128, 2, D], BF16)
    zt_b = Buf("zt")
    dve(lambda e: e.memset(zt[:], 0.0), w=[zt_b])
    with ExitStack() as ss:
        pos_i = salloc(ss, "pos_i", [128, S], I32)
        ang = salloc(ss, "ang", [128, S], F32)
        kf = salloc(ss, "kf", [128, S], F32)
        ki = salloc(ss, "ki", [128, S], I32)
        rr = salloc(ss, "rr", [128, S], F32)
        r2 = salloc(ss, "r2", [128, S], F32)
        tb = salloc(ss, "tb", [128, S], BF16)
        tb2 = salloc(ss, "tb2", [128, S], BF16)
        b_pos, b_ang, b_kf, b_ki, b_rr, b_r2, b_tb, b_tb2 = [Buf(n) for n in "pos ang kf ki rr r2 tb tb2".split()]
        k.dma("sp", lambda e: e.dma_start(out=pos_i[:], in_=din["positions"].ap().partition_broadcast(128)), writes=[b_pos])
        dve(lambda e: e.tensor_copy(out=ang[:], in_=pos_i[:]), r=[b_pos], w=[b_ang])
        dve(lambda e: e.tensor_scalar(out=ang[:], in0=ang[:], scalar1=inv_t[:, 0:1], scalar2=None, op0=ALU.mult), r=[b_ang, inv_b], w=[b_ang])
        dve(lambda e: e.tensor_scalar(out=ki[:], in0=ang[:], scalar1=float(1.0 / TWO_PI), scalar2=None, op0=ALU.mult), r=[b_ang], w=[b_ki])
        dve(lambda e: e.tensor_copy(out=kf[:], in_=ki[:]), r=[b_ki], w=[b_kf])
        dve(lambda e: e.scalar_tensor_tensor(out=rr[:], in0=kf[:], scalar=-C1, in1=ang[:], op0=ALU.mult, op1=ALU.add), r=[b_kf, b_ang], w=[b_rr])
        dve(lambda e: e.scalar_tensor_tensor(out=rr[:], in0=kf[:], scalar=-C2, in1=rr[:], op0=ALU.mult, op1=ALU.add), r=[b_kf, b_rr], w=[b_rr])
        dve(lambda e: e.tensor_scalar(out=r2[:], in0=rr[:], scalar1=float(np.pi / 2), scalar2=None, op0=ALU.add), r=[b_rr], w=[b_r2])
        dve(lambda e: e.tensor_scalar(out=kf[:], in0=r2[:], scalar1=float(np.pi), scalar2=None, op0=ALU.is_gt), r=[b_r2], w=[b_kf])
        dve(lambda e: e.scalar_tensor_tensor(out=r2[:], in0=kf[:], scalar=-TWO_PI, in1=r2[:], op0=ALU.mult, op1=ALU.add), r=[b_kf, b_r2], w=[b_r2])
        dve(lambda e: e.tensor_scalar(out=rr[:], in0=rr[:], scalar1=PI_SAFE, scalar2=-PI_SAFE, op0=ALU.min, op1=ALU.max), r=[b_rr], w=[b_rr])
        dve(lambda e: e.tensor_scalar(out=r2[:], in0=r2[:], scalar1=PI_SAFE, scalar2=-PI_SAFE, op0=ALU.min, op1=ALU.max), r=[b_r2], w=[b_r2])
        act(lambda e: e.activation(out=tb[:], in_=r2[:], func=AF.Sin), r=[b_r2], w=[b_tb])
        act(lambda e: e.activation(out=tb2[:], in_=rr[:], func=AF.Sin), r=[b_rr], w=[b_tb2])
        k.dma("sp", lambda e: e.dma_start(out=tab_scr.ap()[0], in_=tb[:]), reads=[b_tb])
        k.dma("sp", lambda e: e.dma_start(out=tab_scr.ap()[1], in_=tb2[:]), reads=[b_tb2])
        k.barrier()

    xs_v = xs_scr.ap().rearrange("(n p) d -> p n d", p=128)
    zf_evs = []
    for i in range((NSLOT + 128) // 128 // 2 + 0):
        zf_evs.append(k.dma("poolz", lambda e: e.dma_start(out=xs_v[:, i * 2:(i + 1) * 2, :], in_=zt[:]), reads=[zt_b]))
    if (NSLOT + 128) // 128 % 2:
        n_ = (NSLOT + 128) // 128 - 1
        zf_evs.append(k.dma("poolz", lambda e: e.dma_start(out=xs_v[:, n_:n_ + 1, :], in_=zt[:, 0:1, :]), reads=[zt_b]))
    zf_evs.append(k.dma("poolz", lambda e: e.dma_start(out=ys_scr.ap()[NSLOT:NSLOT + 128, :], in_=zt[:, 0, :]), reads=[zt_b]))
    x_d = din["x"].ap()
    win = din["w_in"].ap()

    def wslice(ap2d, c0, n):
        return ap2d[:, c0:c0 + n].rearrange("(c p) n -> p c n", p=128)

    def rstd_ops(stat, stat_b, inv_n, eps):
        act(lambda e: e.activation(out=stat[:, 1:2], in_=stat[:, 0:1], func=AF.Ln, scale=inv_n, bias=eps_ap(eps)), r=[stat_b, epsb], w=[stat_b])
        act(lambda e: e.activation(out=stat[:, 2:3], in_=stat[:, 1:2], func=AF.Exp, scale=-0.5), r=[stat_b], w=[stat_b])

    epst = A("epst", [128, 4], F32)
    epsb = Buf("epst")
    dve(lambda e: e.memset(epst[:, 0:1], EPS), w=[epsb])
    dve(lambda e: e.memset(epst[:, 1:2], LN_EPS), w=[epsb])
    dve(lambda e: e.memset(epst[:, 2:3], 1.0), w=[epsb])
    one_ap = epst[:, 2:3]

    def eps_ap(eps):
        return epst[:, 0:1] if eps == EPS else epst[:, 1:2]

    for s in range(nseq):
        with ExitStack() as a1:
            hT = salloc(a1, "hT", [128, 8, S], BF16)
            hT_b = [Buf("hT%d" % i) for i in range(NCH)]
            cos_t = salloc(a1, "cos_t", [128, S], BF16)
            sin_t = salloc(a1, "sin_t", [128, S], BF16)
            tab_b = Buf("tab")
            k.dma("sp", lambda e: e.dma_start(out=cos_t[:], in_=tab_scr.ap()[0]), writes=[tab_b])
            k.dma("sp", lambda e: e.dma_start(out=sin_t[:], in_=tab_scr.ap()[1]), writes=[tab_b])
            k.barrier()
            with ExitStack() as sn:
                NR = 4
                xt = [salloc(sn, "xt%d" % i, [128, D], F32) for i in range(NR)]
                xt_b = [Buf("xt%d" % i) for i in range(NR)]
                hb = [salloc(sn, "hb%d" % i, [128, D], BF16) for i in range(NR)]
                hb_b = [Buf("hb%d" % i) for i in range(NR)]
                stt = [salloc(sn, "stt%d" % i, [128, 4], F32) for i in range(NR)]
                stt_b = [Buf("stt%d" % i) for i in range(NR)]

                def n_load(t):
                    r0 = s * S + t * 128
                    k.dma("sp", lambda e: e.dma_start(out=xt[t % NR][:], in_=x_d[r0:r0 + 128, :]), writes=[xt_b[t % NR]])

                def n_gen(t):
                    i = t % NR
                    pb = t % 4
                    act(lambda e: e.activation(out=hb[i][:], in_=xt[i][:], func=AF.Square, accum_out=stt[i][:, 0:1]),
                        r=[xt_b[i]], w=[hb_b[i], stt_b[i]])
                    yield
                    act(lambda e: e.activation(out=stt[i][:, 1:2], in_=stt[i][:, 0:1], func=AF.Ln, scale=1.0 / D, bias=eps_ap(EPS)), r=[stt_b[i], epsb], w=[stt_b[i]])
                    yield
                    act(lambda e: e.activation(out=stt[i][:, 2:3], in_=stt[i][:, 1:2], func=AF.Exp, scale=-0.5), r=[stt_b[i]], w=[stt_b[i]])
                    yield
                    act(lambda e: e.activation(out=hb[i][:], in_=xt[i][:], func=AF.Copy, scale=stt[i][:, 2:3]),
                        r=[xt_b[i], stt_b[i]], w=[hb_b[i]])
                    yield
                    for c in range(8):
                        pe(lambda e: e.transpose(psbf[pb][:, c * 128:(c + 1) * 128], hb[i][:, c * 128:(c + 1) * 128], ident_b[:]),
                           r=[hb_b[i], ident_b_b], w=[psb[pb]], inc=(c == 7))
                    yield
                    dve(lambda e: e.tensor_tensor(out=hT[:, :, t * 128:(t + 1) * 128],
                                                  in0=psbf[pb][:, :].rearrange("p (c t) -> p c t", c=8),
                                                  in1=g_mix[:, :].unsqueeze(2).to_broadcast([128, 8, 128]), op=ALU.mult),
                        r=[psb[pb], g_mix_b], w=[hT_b[t // 4]])

                for t in range(min(NR, NT)):
                    n_load(t)
                nact = []
                nn = 0
                while nn < NT or nact:
                    while len(nact) < 3 and nn < NT:
                        nact.append((n_gen(nn), nn))
                        nn += 1
                    for g_ in list(nact):
                        try:
                            next(g_[0])
                        except StopIteration:
                            nact.remove(g_)
                            if g_[1] + NR < NT:
                                n_load(g_[1] + NR)
                k.barrier()
            if dbg:
                k.dma("sp", lambda e: e.dma_start(out=dbg_d["d_hT"].ap()[s], in_=hT[:]), reads=hT_b)
            if stage <= 1:
                k.barrier()
                continue
            with ExitStack() as sc:
                wconv = salloc(sc, "wconv", [128, 8, 512], BF16)
                wconv_b = Buf("wconv")
                k.dma("pool", lambda e: e.dma_start(out=wconv[:], in_=wslice(win, 0, 512)), writes=[wconv_b])
                cT = salloc(sc, "cT", [128, 2, S + 32], BF16)
                cT_b = Buf("cT")
                diag = salloc(sc, "diag", [128, 2, 31, 128], BF16)
                diag_b = Buf("diag")
                tmpf = [salloc(sc, "tmpf%d" % i, [128, 512], F32) for i in range(10)]
                tmpf_b = [Buf("tmpf%d" % i) for i in range(10)]
                pool(lambda e: e.memset(cT[:, :, 0:15], 0.0), w=[cT_b])
                pool(lambda e: e.memset(cT[:, :, S + 15:S + 32], 0.0), w=[cT_b])
                for c2 in range(2):
                    for kk in range(31):
                        dve(lambda e: e.tensor_scalar(out=diag[:, c2, kk, :], in0=ident_f[:], scalar1=wdw[:, c2, kk:kk + 1], scalar2=None, op0=ALU.mult),
                            r=[ident_f_b, wdw_b], w=[diag_b])
                for ch in range(NCH):
                    cols = slice(ch * 512, (ch + 1) * 512)
                    base = (ch % 2) * 4
                    for j in range(4):
                        for c in range(8):
                            pe(lambda e: e.matmul(ps[base + j][:], lhsT=wconv[:, c, j * 128:(j + 1) * 128], rhs=hT[:, c, cols], start=(c == 0), stop=(c == 7)),
                               r=[wconv_b, hT_b[ch]], w=[psb[base + j]], inc=(c == 7))
                    for c2 in range(2):
                        tf, tfb = tmpf[c2], tmpf_b[c2]
                        act(lambda e: e.activation(out=tf[:], in_=ps[base + 2 + c2][:], func=AF.Exp, scale=-1.0), r=[psb[base + 2 + c2]], w=[tfb])
                        act(lambda e: e.activation(out=tf[:], in_=tf[:], func=AF.Ln, bias=one_ap), r=[tfb, epsb], w=[tfb])
                        act(lambda e: e.activation(out=tf[:], in_=tf[:], func=AF.Exp, scale=-1.0), r=[tfb], w=[tfb])
                        dve(lambda e: e.tensor_tensor(out=cT[:, c2, 15 + ch * 512:15 + (ch + 1) * 512], in0=ps[base + c2][:], in1=tf[:], op=ALU.mult),
                            r=[psb[base + c2], tfb], w=[cT_b])
                for ch in range(NCH):
                    cols = slice(ch * 512, (ch + 1) * 512)
                    base = (ch % 2) * 4
                    yb = [tmpf[0], tmpf[1]]
                    ybb = [tmpf_b[0], tmpf_b[1]]
                    ysq = [tmpf[2], tmpf[3]]
                    ysqb = [tmpf_b[2], tmpf_b[3]]
                    for c2 in range(2):
                        for kk in range(31):
                            pe(lambda e: e.matmul(ps[base + c2][:], lhsT=diag[:, c2, kk, :], rhs=cT[:, c2, ch * 512 + kk:ch * 512 + kk + 512], start=(kk == 0), stop=(kk == 30)),
                               r=[diag_b, cT_b], w=[psb[base + c2]], inc=(kk == 30))
                        act(lambda e: e.activation(out=yb[c2][:], in_=ps[base + c2][:], func=AF.Identity, bias=cv_b[:, c2:c2 + 1]),
                            r=[psb[base + c2], cv_b_b], w=[ybb[c2]])
                        act(lambda e: e.activation(out=ysq[c2][:], in_=yb[c2][:], func=AF.Square), r=[ybb[c2]], w=[ysqb[c2]])
                    for c2 in range(2):
                        pe(lambda e: e.matmul(ps[base + 2][:], lhsT=ones_f[:], rhs=yb[c2][:], start=(c2 == 0), stop=(c2 == 1)),
                           r=[ones_f_b, ybb[c2]], w=[psb[base + 2]], inc=(c2 == 1))
                    for c2 in range(2):
                        pe(lambda e: e.matmul(ps[base + 3][:], lhsT=ones_f[:], rhs=ysq[c2][:], start=(c2 == 0), stop=(c2 == 1)),
                           r=[ones_f_b, ysqb[c2]], w=[psb[base + 3]], inc=(c2 == 1))
                    mean_s, mean_sb = tmpf[4], tmpf_b[4]
                    var_s, var_sb = tmpf[5], tmpf_b[5]
                    act(lambda e: e.activation(out=mean_s[:], in_=ps[base + 2][:], func=AF.Copy, scale=1.0 / 256), r=[psb[base + 2]], w=[mean_sb])
                    dve(lambda e: e.tensor_tensor(out=var_s[:], in0=mean_s[:], in1=mean_s[:], op=ALU.mult), r=[mean_sb], w=[var_sb])
                    dve(lambda e: e.scalar_tensor_tensor(out=var_s[:], in0=ps[base + 3][:], scalar=1.0 / 256, in1=var_s[:], op0=ALU.mult, op1=ALU.subtract),
                        r=[psb[base + 3], var_sb], w=[var_sb])
                    act(lambda e: e.activation(out=var_s[:], in_=var_s[:], func=AF.Ln, bias=eps_ap(LN_EPS)), r=[var_sb, epsb], w=[var_sb])
                    act(lambda e: e.activation(out=var_s[:], in_=var_s[:], func=AF.Exp, scale=-0.5), r=[var_sb], w=[var_sb])
                    for c2 in range(2):
                        dd, ddb = tmpf[6 + c2], tmpf_b[6 + c2]
                        ee, eeb = tmpf[8 + c2], tmpf_b[8 + c2]
                        dve(lambda e: e.tensor_tensor(out=dd[:], in0=yb[c2][:], in1=mean_s[:], op=ALU.subtract), r=[ybb[c2], mean_sb], w=[ddb])
                        dve(lambda e: e.tensor_tensor(out=dd[:], in0=dd[:], in1=var_s[:], op=ALU.mult), r=[ddb, var_sb], w=[ddb])
                        dve(lambda e: e.tensor_scalar(out=dd[:], in0=dd[:], scalar1=ln_g[:, c2:c2 + 1], scalar2=ln_b[:, c2:c2 + 1], op0=ALU.mult, op1=ALU.add),
                            r=[ddb, ln_g_b, ln_b_b], w=[ddb])
                        act(lambda e: e.activation(out=ee[:], in_=dd[:], func=AF.Exp, scale=-1.0), r=[ddb], w=[eeb])
                        act(lambda e: e.activation(out=ee[:], in_=ee[:], func=AF.Ln, bias=one_ap), r=[eeb, epsb], w=[eeb])
                        act(lambda e: e.activation(out=ee[:], in_=ee[:], func=AF.Exp, scale=-1.0), r=[eeb], w=[eeb])
                        dve(lambda e: e.tensor_tensor(out=zT[:, c2, cols], in0=dd[:], in1=ee[:], op=ALU.mult), r=[ddb, eeb], w=[zT_b[ch]])
                k.barrier()
            if dbg:
                k.dma("sp", lambda e: e.dma_start(out=dbg_d["d_zT"].ap()[s], in_=zT[:]), reads=zT_b)
            if stage <= 2:
                k.barrier()
                continue
            with ExitStack() as sh:
                wq = [salloc(sh, "wq%d" % i, [128, 8, 384], BF16) for i in range(2)]
                wq_b = [[Buf("wq%d_%d" % (i, j)) for j in range(3)] for i in range(2)]
                qT = salloc(sh, "qT", [128, S], BF16)
                kT = salloc(sh, "kT", [128, S], BF16)
                vT = salloc(sh, "vT", [128, S], BF16)
                qT_b = [Buf("qT%d" % i) for i in range(NCH)]
                kT_b = [Buf("kT%d" % i) for i in range(NCH)]
                vT_b = [Buf("vT%d" % i) for i in range(NCH)]
                acc = [salloc(sh, "acc%d" % i, [128, S], F32) for i in range(2)]
                acc_b = [Buf("acc%d" % i) for i in range(2)]
                qraw = [salloc(sh, "qraw%d" % i, [128, 512], BF16) for i in range(2)]
                qraw_b = [Buf("qraw%d" % i) for i in range(2)]
                t1 = [salloc(sh, "t1_%d" % i, [128, 512], F32) for i in range(2)]
                t1_b = [Buf("t1_%d" % i) for i in range(2)]
                t2 = [salloc(sh, "t2_%d" % i, [128, 512], F32) for i in range(2)]
                t2_b = [Buf("t2_%d" % i) for i in range(2)]
                RP = 4
                Pt = [salloc(sh, "Pt%d" % i, [128, 2, 256], BF16) for i in range(RP)]
                Pt_b = [Buf("Pt%d" % i) for i in range(RP)]
                vaug = [salloc(sh, "vaug%d" % i, [128, 2, 66], BF16) for i in range(RP)]
                vaug_b = [Buf("vaug%d" % i) for i in range(RP)]
                rden = [salloc(sh, "rden%d" % i, [128, 512], F32) for i in range(2)]
                rden_b = [Buf("rden%d" % i) for i in range(2)]
                oTc = [salloc(sh, "oTc%d" % i, [128, 512], BF16) for i in range(4)]
                oTc_b = [Buf("oTc%d" % i) for i in range(4)]
                for i in range(RP):
                    dve(lambda e: e.memset(vaug[i][:], 1.0), w=[vaug_b[i]])
                OBK = (5, 6)
                ob_b = [psb[5], psb[6]]
                BV = 4
                rq = 0
                for p in range(lim.get('pairs', 6)):
                    wb = p % 2
                    for j, c0 in enumerate((512 + 128 * p, 512 + 768 + 128 * p, 512 + 1536 + 128 * p)):
                        k.dma("pool", lambda e: e.dma_start(out=wq[wb][:, :, j * 128:(j + 1) * 128], in_=wslice(win, c0, 128)), writes=[wq_b[wb][j]])
                    for ch in range(NCH if lim.get('proj', 1) else 0):
                        cols = slice(ch * 512, (ch + 1) * 512)
                        banks = (5, 6, 7)
                        for j in range(3):
                            for c in range(8):
                                pe(lambda e: e.matmul(ps[banks[j]][:], lhsT=wq[wb][:, c, j * 128:(j + 1) * 128], rhs=hT[:, c, cols], start=(c == 0), stop=(c == 7)),
                                   r=[wq_b[wb][j], hT_b[ch]], w=[psb[banks[j]]], inc=(c == 7))
                        for j in range(2):
                            act(lambda e: e.activation(out=qraw[j][:], in_=ps[banks[j]][:], func=AF.Copy), r=[psb[banks[j]]], w=[qraw_b[j]])
                        act(lambda e: e.activation(out=vT[:, cols], in_=ps[banks[2]][:], func=AF.Copy), r=[psb[banks[2]]], w=[vT_b[ch]])
                        for j, (dst, dst_b) in enumerate(((qT, qT_b), (kT, kT_b))):
                            bank2 = (4, 3)[j]
                            pe(lambda e: e.matmul(ps[bank2][:], lhsT=rot_b[:], rhs=qraw[j][:], start=True, stop=True),
                               r=[rot_b_b, qraw_b[j]], w=[psb[bank2]])
                            dve(lambda e: e.tensor_tensor(out=t1[j][:], in0=ps[banks[j]][:], in1=cos_t[:, cols], op=ALU.mult), r=[psb[banks[j]], tab_b], w=[t1_b[j]])
                            dve(lambda e: e.tensor_tensor(out=t2[j][:], in0=ps[bank2][:], in1=sin_t[:, cols], op=ALU.mult), r=[psb[bank2], tab_b], w=[t2_b[j]])
                            dve(lambda e: e.tensor_tensor(out=dst[:, cols], in0=t1[j][:], in1=t2[j][:], op=ALU.add), r=[t1_b[j], t2_b[j]], w=[dst_b[ch]])
                    tiles = []
                    for bi, dil in enumerate((1, 4, 16)[:lim.get('branches', 3)]):
                        L = S // dil
                        NB = L // 128
                        for r in range(dil):
                            for j in range(NB + 1):
                                tiles.append((bi, dil, NB, r, j))

                    def a_front(ti):
                        bi, dil, NB, r, j = tiles[ti]
                        edge_f, edge_l = (j == 0), (j == NB)
                        nk = 64 if (edge_f or edge_l) else 128
                        ak0 = 0 if edge_f else 128 * j - 64
                        blocks = [b for b in (j - 1, j) if 0 <= b < NB]
                        aq0 = 128 * blocks[0]
                        nq = 128 * len(blocks)
                        ksl = slice(r + dil * ak0, r + dil * (ak0 + nk - 1) + 1, dil)
                        qsl = slice(r + dil * aq0, r + dil * (aq0 + nq - 1) + 1, dil)
                        tk0, tk1 = r + dil * ak0, r + dil * (ak0 + nk - 1)
                        tq0, tq1 = r + dil * aq0, r + dil * (aq0 + nq - 1)
                        kdeps = [kT_b[c] for c in range(tk0 // 512, tk1 // 512 + 1)]
                        vdeps = [vT_b[c] for c in range(tk0 // 512, tk1 // 512 + 1)]
                        qdeps = [qT_b[c] for c in range(tq0 // 512, tq1 // 512 + 1)]
                        vi = ti % RP
                        sdb = ti % 2
                        pe(lambda e: e.transpose(psbf[BV][0:nk, 0:128], vT[:, ksl], ident_b[:]), r=vdeps + [ident_b_b], w=[psb[BV]])
                        act(lambda e: e.activation(out=vaug[vi][0:nk, :, 0:64], in_=psbf[BV][0:nk, 0:128].rearrange("p (h d) -> p h d", h=2), func=AF.Copy),
                            r=[psb[BV]], w=[vaug_b[vi]])
                        for h in range(2):
                            pe(lambda e: e.matmul(ps[2 * sdb + h][0:nk, 0:nq], lhsT=kT[64 * h:64 * h + 64, ksl], rhs=qT[64 * h:64 * h + 64, qsl], start=True, stop=True),
                               r=kdeps + qdeps, w=[psb[2 * sdb + h]], inc=(h == 1))
                        act(lambda e: e.activation(out=Pt[vi][0:nk, :, 0:nq], in_=ps2[sdb][0:nk, :].rearrange("p (h q) -> p h q", h=2)[:, :, 0:nq], func=AF.Exp, scale=0.125),
                            r=[psb[2 * sdb], psb[2 * sdb + 1]], w=[Pt_b[vi]])
                        if edge_f:
                            mk, mkb = maskf[0:64, 0:128], maskf_b
                        elif edge_l:
                            mk, mkb = masklh[0:64, 0:128], masklh_b
                        else:
                            mk, mkb = masklh[:, 0:256], masklh_b
                        dve(lambda e: e.tensor_tensor(out=Pt[vi][0:nk, :, 0:nq], in0=Pt[vi][0:nk, :, 0:nq], in1=mk.unsqueeze(1).to_broadcast([nk, 2, nq]), op=ALU.mult),
                            r=[Pt_b[vi], mkb], w=[Pt_b[vi]])

                    def a_back(ti):
                        bi, dil, NB, r, j = tiles[ti]
                        nk = 64 if (j == 0 or j == NB) else 128
                        blocks = [b for b in (j - 1, j) if 0 <= b < NB]
                        vi = ti % RP
                        for bi2, b in enumerate(blocks):
                            for h in range(2):
                                oc0 = h * 128
                                pe(lambda e: e.matmul(ps[OBK[b % 2]][0:65, oc0:oc0 + 128], lhsT=vaug[vi][0:nk, h, 0:65], rhs=Pt[vi][0:nk, h, bi2 * 128:(bi2 + 1) * 128],
                                                      start=(j == b and h == 0), stop=(j == b + 1), skip_group_check=True),
                                   r=[vaug_b[vi], Pt_b[vi]], w=[ob_b[b % 2]], inc=(h == 1))
                        if j >= 1:
                            b = j - 1
                            tsl = slice(r + dil * 128 * b, r + dil * (128 * b + 127) + 1, dil)
                            for h in range(2):
                                oc0 = h * 128
                                if bi == 0:
                                    dve(lambda e: e.tensor_copy(out=acc[h][0:65, tsl], in_=ps[OBK[b % 2]][0:65, oc0:oc0 + 128]), r=[ob_b[b % 2]], w=[acc_b[h]])
                                else:
                                    dve(lambda e: e.tensor_tensor(out=acc[h][0:65, tsl], in0=acc[h][0:65, tsl], in1=ps[OBK[b % 2]][0:65, oc0:oc0 + 128], op=ALU.add),
                                        r=[ob_b[b % 2], acc_b[h]], w=[acc_b[h]])

                    PFA = 2
                    for ti in range(min(PFA, len(tiles))):
                        a_front(ti)
                    for ti in range(len(tiles)):
                        if ti + PFA < len(tiles):
                            a_front(ti + PFA)
                        a_back(ti)
                    for ch in range(NCH if lim.get('norm', 1) else 0):
                        cols = slice(ch * 512, (ch + 1) * 512)
                        for h in range(2):
                            i2 = (ch * 2 + h) % 2
                            i4 = (ch * 2 + h) % 4
                            bank = (7, 3)[i2]
                            pe(lambda e: e.matmul(ps[bank][0:64, :], lhsT=esel[0:65, 0:64], rhs=acc[h][0:65, cols], start=True, stop=True),
                               r=[esel_b, acc_b[h]], w=[psb[bank]])
                            act(lambda e: e.activation(out=rden[i2][0:64, :], in_=ps[bank][0:64, :], func=AF.Ln), r=[psb[bank]], w=[rden_b[i2]])
                            act(lambda e: e.activation(out=rden[i2][0:64, :], in_=rden[i2][0:64, :], func=AF.Exp, scale=-1.0), r=[rden_b[i2]], w=[rden_b[i2]])
                            dve(lambda e: e.tensor_tensor(out=oTc[i4][0:64, :], in0=acc[h][0:64, cols], in1=rden[i2][0:64, :], op=ALU.mult),
                                r=[acc_b[h], rden_b[i2]], w=[oTc_b[i4]])
                            row0 = p * 128 + h * 64
                            k.dma("sp", lambda e: e.dma_start(out=o_scr.ap()[s, row0:row0 + 64, cols], in_=oTc[i4][0:64, :]), reads=[oTc_b[i4]])
                k.barrier()
        if stage <= 3:
            k.barrier()
            continue
        k.join_detached()
        with ExitStack() as a2:
            wo = salloc(a2, "wo", [128, 8, D], BF16)
            wxq = salloc(a2, "wxq", [128, 8, D], BF16)
            wxo = salloc(a2, "wxo", [128, 8, D], BF16)
            wo_b, wxq_b, wxo_b = Buf("wo"), Buf("wxq"), Buf("wxo")
            KTm = salloc(a2, "KTm", [128, 8, MEM], BF16)
            Vm = salloc(a2, "Vm", [128, 2, D], BF16)
            KTm_b, Vm_b = Buf("KTm"), Buf("Vm")

            def wfull(name):
                return din[name].ap().rearrange("(c p) n -> p c n", p=128)

            k.dma("pool", lambda e: e.dma_start(out=wo[:], in_=wfull("w_out")), writes=[wo_b])
            with ExitStack() as am:
                wxk = salloc(am, "wxk", [128, 8, D], BF16)
                wxv = salloc(am, "wxv", [128, 8, D], BF16)
                wxk_b, wxv_b = Buf("wxk"), Buf("wxv")
                k.dma("pool", lambda e: e.dma_start(out=wxk[:], in_=wfull("w_xk")), writes=[wxk_b])
                k.dma("pool", lambda e: e.dma_start(out=wxv[:], in_=wfull("w_xv")), writes=[wxv_b])
                k.dma("pool", lambda e: e.dma_start(out=wxq[:], in_=wfull("w_xq")), writes=[wxq_b])
                k.dma("pool", lambda e: e.dma_start(out=wxo[:], in_=wfull("w_xo")), writes=[wxo_b])
                mt = [salloc(am, "mt%d" % i, [128, D], F32) for i in range(2)]
                mt_b = [Buf("mt%d" % i) for i in range(2)]
                mb = [salloc(am, "mb%d" % i, [128, D], BF16) for i in range(2)]
                mb_b = [Buf("mb%d" % i) for i in range(2)]
                mst = [salloc(am, "mst%d" % i, [128, 4], F32) for i in range(2)]
                mst_b = [Buf("mst%d" % i) for i in range(2)]
                memT = salloc(am, "memT", [128, 8, MEM], BF16)
                memT_b = Buf("memT")
                for m in range(2):
                    r0 = s * MEM + m * 128
                    k.dma("sp", lambda e: e.dma_start(out=mt[m][:], in_=din["mem"].ap()[r0:r0 + 128, :]), writes=[mt_b[m]])
                    act(lambda e: e.activation(out=mb[m][:], in_=mt[m][:], func=AF.Square, accum_out=mst[m][:, 0:1]), r=[mt_b[m]], w=[mb_b[m], mst_b[m]])
                    rstd_ops(mst[m], mst_b[m], 1.0 / D, EPS)
                    act(lambda e: e.activation(out=mb[m][:], in_=mt[m][:], func=AF.Copy, scale=mst[m][:, 2:3]), r=[mt_b[m], mst_b[m]], w=[mb_b[m]])
                    for c in range(8):
                        pe(lambda e: e.transpose(psbf[m][:, c * 128:(c + 1) * 128], mb[m][:, c * 128:(c + 1) * 128], ident_b[:]),
                           r=[mb_b[m], ident_b_b], w=[psb[m]], inc=(c == 7))
                    dve(lambda e: e.tensor_tensor(out=memT[:, :, m * 128:(m + 1) * 128], in0=psbf[m][:, :].rearrange("p (c t) -> p c t", c=8),
                                                  in1=g_mem[:, :].unsqueeze(2).to_broadcast([128, 8, 128]), op=ALU.mult),
                        r=[psb[m], g_mem_b], w=[memT_b])
                for oc in range(8):
                    bank = 2 + oc % 4
                    for c in range(8):
                        pe(lambda e: e.matmul(ps[bank][:, 0:MEM], lhsT=wxk[:, c, oc * 128:(oc + 1) * 128], rhs=memT[:, c, :], start=(c == 0), stop=(c == 7)),
                           r=[wxk_b, memT_b], w=[psb[bank]], inc=(c == 7))
                    act(lambda e: e.activation(out=KTm[:, oc, :], in_=ps[bank][:, 0:MEM], func=AF.Copy), r=[psb[bank]], w=[KTm_b])
                for m in range(2):
                    for half in range(2):
                        bank = 2 + (m * 2 + half)
                        for c in range(8):
                            pe(lambda e: e.matmul(ps[bank][:], lhsT=memT[:, c, m * 128:(m + 1) * 128], rhs=wxv[:, c, half * 512:(half + 1) * 512], start=(c == 0), stop=(c == 7)),
                               r=[wxv_b, memT_b], w=[psb[bank]], inc=(c == 7))
                        act(lambda e: e.activation(out=Vm[:, m, half * 512:(half + 1) * 512], in_=ps[bank][:], func=AF.Copy), r=[psb[bank]], w=[Vm_b])
                k.barrier()

            lgall = salloc(a2, "lgall", [128, NT, 36], F32)
            lgall_b = Buf("lgall")
            a2t = ExitStack()

            def ring(name, shape, dtype, n=2):
                return ([salloc(a2t, "%s%d" % (name, i), shape, dtype) for i in range(n)], [Buf("%s%d" % (name, i)) for i in range(n)])

            xt2, xt2_b = ring("xt2", [128, D], F32, 4)
            oTt, oTt_b = ring("oTt", [128, 6, 128], BF16, 4)
            sqb, sqb_b = ring("sqb", [128, 8, 128], BF16, 3)
            lnb, lnb_b = ring("lnb", [128, 2, 128], F32, 3)
            rsb, rsb_b = ring("rsb", [128, 2, 128], F32, 3)
            mixg, mixg_b = ring("mixg", [128, 8, 128], BF16, 3)
            st2, st2_b = ring("st2", [128, 8], F32, 3)
            x1, x1_b = ring("x1", [128, D], F32, 3)
            h2b, h2b_b = ring("h2b", [128, D], BF16, 3)
            h2T, h2T_b = ring("h2T", [128, 8, 128], BF16, 3)
            qxT, qxT_b = ring("qxT", [128, 8, 128], BF16, 3)
            Pm, Pm_b = ring("Pm", [128, 2, 4, 128], BF16, 3)
            rdx, rdx_b = ring("rdx", [128, 4, 128], F32, 3)
            oxT, oxT_b = ring("oxT", [128, 8, 128], BF16, 3)
            x2, x2_b = x1, x1_b
            h3b, h3b_b = ring("h3b", [128, D], BF16, 4)
            h3T, h3T_b = ring("h3T", [128, 8, 128], BF16, 3)

            def t_load(t):
                i4 = t % 4
                r0 = s * S + t * 128
                tok = slice(t * 128, (t + 1) * 128)
                k.dma("sp", lambda e: e.dma_start(out=xt2[i4][:], in_=x_d[r0:r0 + 128, :]), writes=[xt2_b[i4]])
                k.dma("sp", lambda e: e.dma_start(out=oTt[i4][:], in_=o_scr.ap()[s, :, tok].rearrange("(c p) t -> p c t", p=128)), writes=[oTt_b[i4]])

            def tile_gen(t):
                i = t % 3
                i4 = t % 4
                Ba, Bb = 2 * i, 2 * i + 1
                BK = (Ba, Bb)
                tt = s * NT + t
                r0 = s * S + t * 128
                tok = slice(t * 128, (t + 1) * 128)
                zb = zT_b[t // 4]
                act(lambda e: e.activation(out=sqb[i][:, 0:2, :], in_=zT[:, :, tok], func=AF.Square), r=[zb], w=[sqb_b[i]])
                act(lambda e: e.activation(out=sqb[i][:, 2:8, :], in_=oTt[i4][:], func=AF.Square), r=[oTt_b[i4]], w=[sqb_b[i]])
                yield
                for c in range(8):
                    col = 0 if c < 2 else 128
                    pe(lambda e: e.matmul(ps[Ba][:, col:col + 128], lhsT=ones_b[:], rhs=sqb[i][:, c, :], start=(c == 0), stop=(c == 1 or c == 7), skip_group_check=True),
                       r=[sqb_b[i], ones_b_b], w=[psb[Ba]], inc=(c == 7))
                yield
                act(lambda e: e.activation(out=lnb[i][:, 0, :], in_=ps[Ba][:, 0:128], func=AF.Ln, scale=1.0 / 256, bias=eps_ap(EPS)), r=[psb[Ba], epsb], w=[lnb_b[i]])
                act(lambda e: e.activation(out=lnb[i][:, 1, :], in_=ps[Ba][:, 128:256], func=AF.Ln, scale=1.0 / 768, bias=eps_ap(EPS)), r=[psb[Ba], epsb], w=[lnb_b[i]])
                act(lambda e: e.activation(out=rsb[i][:], in_=lnb[i][:], func=AF.Exp, scale=-0.5), r=[lnb_b[i]], w=[rsb_b[i]])
                yield
                for c in range(8):
                    src = zT[:, c, tok] if c < 2 else oTt[i4][:, c - 2, :]
                    k.op("dve", lambda e: e.scalar_tensor_tensor(out=mixg[i][:, c, :], in0=src, scalar=g_mx[:, c:c + 1], in1=rsb[i][:, 0 if c < 2 else 1, :], op0=ALU.mult, op1=ALU.mult),
                         [zb, oTt_b[i4], g_mx_b, rsb_b[i]], [mixg_b[i]], inc=(c == 7))
                yield
                for half in range(2):
                    hs = slice(half * 512, (half + 1) * 512)
                    for c in range(8):
                        pe(lambda e: e.matmul(ps[BK[half]][:], lhsT=mixg[i][:, c, :], rhs=wo[:, c, hs], start=(c == 0), stop=(c == 7)),
                           r=[mixg_b[i], wo_b], w=[psb[BK[half]]], inc=(c == 7))
                yield
                for half in range(2):
                    hs = slice(half * 512, (half + 1) * 512)
                    k.op("dve", lambda e: e.tensor_tensor(out=x1[i][:, hs], in0=ps[BK[half]][:], in1=xt2[i4][:, hs], op=ALU.add),
                         [psb[BK[half]], xt2_b[i4]], [x1_b[i]], inc=(half == 1))
                yield
                act(lambda e: e.activation(out=h2b[i][:], in_=x1[i][:], func=AF.Square, accum_out=st2[i][:, 4:5]), r=[x1_b[i]], w=[h2b_b[i], st2_b[i]])
                act(lambda e: e.activation(out=st2[i][:, 5:6], in_=st2[i][:, 4:5], func=AF.Ln, scale=1.0 / D, bias=eps_ap(EPS)), r=[st2_b[i], epsb], w=[st2_b[i]])
                yield
                act(lambda e: e.activation(out=st2[i][:, 6:7], in_=st2[i][:, 5:6], func=AF.Exp, scale=-0.5), r=[st2_b[i]], w=[st2_b[i]])
                act(lambda e: e.activation(out=h2b[i][:], in_=x1[i][:], func=AF.Copy, scale=st2[i][:, 6:7]), r=[x1_b[i], st2_b[i]], w=[h2b_b[i]])
                yield
                for c in range(8):
                    pe(lambda e: e.transpose(psbf[Ba][:, c * 128:(c + 1) * 128], h2b[i][:, c * 128:(c + 1) * 128], ident_b[:]),
                       r=[h2b_b[i], ident_b_b], w=[psb[Ba]], inc=(c == 7))
                yield
                dve(lambda e: e.tensor_tensor(out=h2T[i][:], in0=psbf[Ba][:, :].rearrange("p (c t) -> p c t", c=8),
                                              in1=g_xn[:, :].unsqueeze(2).to_broadcast([128, 8, 128]), op=ALU.mult),
                    r=[psb[Ba], g_xn_b], w=[h2T_b[i]])
                yield
                for hb_ in range(2):
                    bank = BK[hb_]
                    for o_ in range(4):
                        oc = hb_ * 4 + o_
                        for c in range(8):
                            pe(lambda e: e.matmul(ps[bank][:, o_ * 128:(o_ + 1) * 128], lhsT=wxq[:, c, oc * 128:(oc + 1) * 128], rhs=h2T[i][:, c, :], start=(c == 0), stop=(c == 7)),
                               r=[wxq_b, h2T_b[i]], w=[psb[bank]], inc=(c == 7 and o_ == 3))
                    yield
                for hb_ in range(2):
                    k.op("act", lambda e: e.activation(out=qxT[i][:, hb_ * 4:(hb_ + 1) * 4, :], in_=ps[BK[hb_]][:].rearrange("p (c t) -> p c t", c=4), func=AF.Copy),
                         [psb[BK[hb_]]], [qxT_b[i]], inc=True)
                yield
                for m in range(2):
                    for hd in range(4):
                        for c in range(2):
                            pe(lambda e: e.matmul(ps[BK[m]][:, hd * 128:(hd + 1) * 128], lhsT=KTm[:, hd * 2 + c, m * 128:(m + 1) * 128], rhs=qxT[i][:, hd * 2 + c, :],
                                                  start=(c == 0), stop=(c == 1)),
                               r=[KTm_b, qxT_b[i]], w=[psb[BK[m]]], inc=(c == 1 and hd == 3))
                yield
                for m in range(2):
                    act(lambda e: e.activation(out=Pm[i][:, m, :, :], in_=ps[BK[m]][:].rearrange("p (h t) -> p h t", h=4), func=AF.Exp, scale=1.0 / 16),
                        r=[psb[BK[m]]], w=[Pm_b[i]])
                yield
                for m in range(2):
                    pe(lambda e: e.matmul(ps[Ba][:], lhsT=ones_b[:], rhs=Pm[i][:, m, :, :], start=(m == 0), stop=(m == 1)),
                       r=[ones_b_b, Pm_b[i]], w=[psb[Ba]], inc=(m == 1))
                yield
                act(lambda e: e.activation(out=rdx[i][:], in_=ps[Ba][:].rearrange("p (h t) -> p h t", h=4), func=AF.Ln), r=[psb[Ba]], w=[rdx_b[i]])
                yield
                act(lambda e: e.activation(out=rdx[i][:], in_=rdx[i][:], func=AF.Exp, scale=-1.0), r=[rdx_b[i]], w=[rdx_b[i]])
                yield
                for hb_ in range(2):
                    bank = BK[hb_]
                    for o_ in range(4):
                        oc = hb_ * 4 + o_
                        hd = oc // 2
                        for m in range(2):
                            pe(lambda e: e.matmul(ps[bank][:, o_ * 128:(o_ + 1) * 128], lhsT=Vm[:, m, oc * 128:(oc + 1) * 128], rhs=Pm[i][:, m, hd, :], start=(m == 0), stop=(m == 1)),
                               r=[Vm_b, Pm_b[i]], w=[psb[bank]], inc=(m == 1 and o_ == 3))
                yield
                yield
                for hb_ in range(2):
                    k.op("dve", lambda e: e.tensor_tensor(out=oxT[i][:, hb_ * 4:(hb_ + 1) * 4, :].rearrange("p (h c) t -> p h c t", h=2),
                                                         in0=ps[BK[hb_]][:].rearrange("p (h c t) -> p h c t", h=2, c=2),
                                                         in1=rdx[i][:, hb_ * 2:(hb_ + 1) * 2, :].unsqueeze(2).to_broadcast([128, 2, 2, 128]), op=ALU.mult),
                         [psb[BK[hb_]], rdx_b[i]], [oxT_b[i]], inc=True)
                yield
                for half in range(2):
                    hs = slice(half * 512, (half + 1) * 512)
                    for c in range(8):
                        pe(lambda e: e.matmul(ps[BK[half]][:], lhsT=oxT[i][:, c, :], rhs=wxo[:, c, hs], start=(c == 0), stop=(c == 7)),
                           r=[oxT_b[i], wxo_b], w=[psb[BK[half]]], inc=(c == 7))
                yield
                for half in range(2):
                    hs = slice(half * 512, (half + 1) * 512)
                    k.op("dve", lambda e: e.tensor_tensor(out=x2[i][:, hs], in0=ps[BK[half]][:], in1=x1[i][:, hs], op=ALU.add),
                         [psb[BK[half]], x1_b[i]], [x2_b[i]], inc=(half == 1))
                k.dma("sp", lambda e: e.dma_start(out=x2_scr.ap()[r0:r0 + 128, :], in_=x2[i][:]), reads=[x2_b[i]])
                yield
                act(lambda e: e.activation(out=h3b[i4][:], in_=x2[i][:], func=AF.Square, accum_out=st2[i][:, 4:5]), r=[x2_b[i]], w=[h3b_b[i4], st2_b[i]])
                act(lambda e: e.activation(out=st2[i][:, 5:6], in_=st2[i][:, 4:5], func=AF.Ln, scale=1.0 / D, bias=eps_ap(EPS)), r=[st2_b[i], epsb], w=[st2_b[i]])
                yield
                act(lambda e: e.activation(out=st2[i][:, 7:8], in_=st2[i][:, 5:6], func=AF.Exp, scale=-0.5), r=[st2_b[i]], w=[st2_b[i]])
                yield
                dve(lambda e: e.scalar_tensor_tensor(out=h3b[i4][:], in0=x2[i][:], scalar=st2[i][:, 7:8], in1=gmoe[:], op0=ALU.mult, op1=ALU.mult),
                    r=[x2_b[i], st2_b[i], gmoe_b], w=[h3b_b[i4]])
                yield
                for c in range(8):
                    pe(lambda e: e.transpose(psbf[Ba][:, c * 128:(c + 1) * 128], h3b[i4][:, c * 128:(c + 1) * 128], ident_b[:]),
                       r=[h3b_b[i4], ident_b_b], w=[psb[Ba]], inc=(c == 7))
                yield
                act(lambda e: e.activation(out=h3T[i][:], in_=psbf[Ba][:, :].rearrange("p (c t) -> p c t", c=8), func=AF.Copy), r=[psb[Ba]], w=[h3T_b[i]])
                yield
                for c in range(8):
                    pe(lambda e: e.matmul(ps[Bb][:, 0:36], lhsT=h3T[i][:, c, :], rhs=wrb[:, c, :], start=(c == 0), stop=(c == 7)),
                       r=[h3T_b[i], wrb_b], w=[psb[Bb]], inc=(c == 7))
                yield
                dve(lambda e: e.tensor_tensor(out=lgall[:, t, :], in0=ps[Bb][:, 0:36], in1=brt[:], op=ALU.add), r=[psb[Bb], brt_b], w=[lgall_b])
                k.dma("sp", lambda e: e.dma_start(out=h3_scr.ap()[r0:r0 + 128, :], in_=h3b[i4][:]), reads=[h3b_b[i4]])

            NL = 3
            for t in range(min(4, NT)):
                t_load(t)
            active = []
            nxt = 0
            while nxt < NT or active:
                while len(active) < NL and nxt < NT:
                    active.append([tile_gen(nxt), nxt])
                    nxt += 1
                for a_ in list(active):
                    try:
                        next(a_[0])
                    except StopIteration:
                        active.remove(a_)
                        if a_[1] + 4 < NT:
                            t_load(a_[1] + 4)
            k.barrier()
            a2t.close()

            with ExitStack() as rs:
                NTl = NT

                def rbuf(name, shape, dtype=F32):
                    return salloc(rs, name, shape, dtype), Buf(name)

                gmx, gmx_b = rbuf("gmx", [128, NTl])
                goh, goh_b = rbuf("goh", [128, NTl, 4])
                gsh, gsh_b = rbuf("gsh", [128, NTl, 4])
                gsm, gsm_b = rbuf("gsm", [128, NTl])
                pgv, pgv_b = rbuf("pgv", [128, NTl])
                tmp4, tmp4_b = rbuf("tmp4", [128, NTl, 32])
                es8, es8_b = rbuf("es8", [128, NTl, 8])
                m1v, m1v_b = rbuf("m1v", [128, NTl])
                m2v, m2v_b = rbuf("m2v", [128, NTl])
                oh1, oh1_b = rbuf("oh1", [128, NTl, 8])
                oh2, oh2_b = rbuf("oh2", [128, NTl, 8])
                el2, el2_b = rbuf("el2", [128, NTl, 8])
                dmv, dmv_b = rbuf("dmv", [128, NTl])
                exv, exv_b = rbuf("exv", [128, NTl])
                w1v, w1v_b = rbuf("w1v", [128, NTl])
                w2v, w2v_b = rbuf("w2v", [128, NTl])
                o32 = [rbuf("o32_%d" % q, [128, NTl, 32]) for q in range(2)]
                selA, selA_b = rbuf("selA", [128, NTl, 32], BF16)
                rnk, rnk_b = rbuf("rnk", [128, NTl, 32])
                cnts, cnts_b = rbuf("cnts", [128, NTl, 32])
                bcum, bcum_b = rbuf("bcum", [128, NTl, 32])
                okv, okv_b = rbuf("okv", [128, NTl, 32])
                slf, slf_b = rbuf("slf", [128, NTl, 2])
                sidxA, sidxA_b = rbuf("sidxA", [128, NTl * 2], I32)
                hrow = [rbuf("hrow%d" % i, [128, D], BF16) for i in range(4)]

                lg3 = lgall[:, :, :]
                gl = lgall[:, :, 0:4]
                el = lgall[:, :, 4:36]
                el4 = el.rearrange("p t (g e) -> p t g e", g=4)
                dve(lambda e: e.tensor_reduce(out=gmx[:], in_=gl, axis=AX.X, op=ALU.max), r=[lgall_b], w=[gmx_b])
                dve(lambda e: e.tensor_tensor(out=goh[:], in0=gl, in1=gmx[:].unsqueeze(2).to_broadcast([128, NTl, 4]), op=ALU.is_equal), r=[lgall_b, gmx_b], w=[goh_b])
                dve(lambda e: e.tensor_tensor(out=gsh[:], in0=gl, in1=gmx[:].unsqueeze(2).to_broadcast([128, NTl, 4]), op=ALU.subtract), r=[lgall_b, gmx_b], w=[gsh_b])
                act(lambda e: e.activation(out=gsh[:], in_=gsh[:], func=AF.Exp), r=[gsh_b], w=[gsh_b])
                dve(lambda e: e.tensor_reduce(out=gsm[:], in_=gsh[:], axis=AX.X, op=ALU.add), r=[gsh_b], w=[gsm_b])
                dve(lambda e: e.reciprocal(out=pgv[:], in_=gsm[:]), r=[gsm_b], w=[pgv_b])
                dve(lambda e: e.tensor_tensor(out=tmp4[:].rearrange("p t (g e) -> p t g e", g=4), in0=el4, in1=goh[:].unsqueeze(3).to_broadcast([128, NTl, 4, 8]), op=ALU.mult),
                    r=[lgall_b, goh_b], w=[tmp4_b])
                dve(lambda e: e.tensor_reduce(out=es8[:], in_=tmp4[:].rearrange("p t (g e) -> p t e g", g=4), axis=AX.X, op=ALU.add), r=[tmp4_b], w=[es8_b])
                dve(lambda e: e.tensor_reduce(out=m1v[:], in_=es8[:], axis=AX.X, op=ALU.max), r=[es8_b], w=[m1v_b])
                dve(lambda e: e.tensor_tensor(out=oh1[:], in0=es8[:], in1=m1v[:].unsqueeze(2).to_broadcast([128, NTl, 8]), op=ALU.is_equal), r=[es8_b, m1v_b], w=[oh1_b])
                dve(lambda e: e.scalar_tensor_tensor(out=el2[:], in0=oh1[:], scalar=-1e30, in1=es8[:], op0=ALU.mult, op1=ALU.add), r=[oh1_b, es8_b], w=[el2_b])
                dve(lambda e: e.tensor_reduce(out=m2v[:], in_=el2[:], axis=AX.X, op=ALU.max), r=[el2_b], w=[m2v_b])
                dve(lambda e: e.tensor_tensor(out=oh2[:], in0=el2[:], in1=m2v[:].unsqueeze(2).to_broadcast([128, NTl, 8]), op=ALU.is_equal), r=[el2_b, m2v_b], w=[oh2_b])
                dve(lambda e: e.tensor_tensor(out=dmv[:], in0=m2v[:], in1=m1v[:], op=ALU.subtract), r=[m1v_b, m2v_b], w=[dmv_b])
                act(lambda e: e.activation(out=exv[:], in_=dmv[:], func=AF.Exp), r=[dmv_b], w=[exv_b])
                dve(lambda e: e.tensor_scalar(out=w1v[:], in0=exv[:], scalar1=1.0, scalar2=None, op0=ALU.add), r=[exv_b], w=[w1v_b])
                dve(lambda e: e.reciprocal(out=w1v[:], in_=w1v[:]), r=[w1v_b], w=[w1v_b])
                dve(lambda e: e.tensor_tensor(out=w2v[:], in0=exv[:], in1=w1v[:], op=ALU.mult), r=[exv_b, w1v_b], w=[w2v_b])
                gview = gate_f[:, s * NT * 2:(s + 1) * NT * 2].rearrange("p (t q) -> p t q", q=2)
                sview = slot_i[:, s * NT * 2:(s + 1) * NT * 2].rearrange("p (t q) -> p t q", q=2)
                dve(lambda e: e.tensor_tensor(out=gview[:, :, 0], in0=w1v[:], in1=pgv[:], op=ALU.mult), r=[w1v_b, pgv_b], w=[rt_b])
                dve(lambda e: e.tensor_tensor(out=gview[:, :, 1], in0=w2v[:], in1=pgv[:], op=ALU.mult), r=[w2v_b, pgv_b], w=[rt_b])
                for q, (ohq, ohq_b) in enumerate(((oh1, oh1_b), (oh2, oh2_b))):
                    dve(lambda e: e.tensor_tensor(out=o32[q][0][:].rearrange("p t (g e) -> p t g e", g=4), in0=goh[:].unsqueeze(3).to_broadcast([128, NTl, 4, 8]),
                                                  in1=ohq[:].unsqueeze(2).to_broadcast([128, NTl, 4, 8]), op=ALU.mult), r=[goh_b, ohq_b], w=[o32[q][1]])
                dve(lambda e: e.tensor_tensor(out=selA[:], in0=o32[0][0][:], in1=o32[1][0][:], op=ALU.add), r=[o32[0][1], o32[1][1]], w=[selA_b])
                for t in range(NTl):
                    bank = t // 16
                    c0 = (t % 16) * 32
                    pe(lambda e: e.matmul(ps[bank][:, c0:c0 + 32], lhsT=tri_b[:], rhs=selA[:, t, :], start=True, stop=True), r=[tri_b_b, selA_b], w=[psb[bank]], inc=False)
                    pe(lambda e: e.matmul(ps[2 + bank][:, c0:c0 + 32], lhsT=ones_b[:], rhs=selA[:, t, :], start=True, stop=True), r=[ones_b_b, selA_b], w=[psb[2 + bank]], inc=(t % 16 == 15))
                for bank in range(2):
                    dve(lambda e: e.tensor_copy(out=rnk[:, bank * 16:(bank + 1) * 16, :], in_=ps[bank][:].rearrange("p (t e) -> p t e", e=32)), r=[psb[bank]], w=[rnk_b])
                    dve(lambda e: e.tensor_copy(out=cnts[:, bank * 16:(bank + 1) * 16, :], in_=ps[2 + bank][:].rearrange("p (t e) -> p t e", e=32)), r=[psb[2 + bank]], w=[cnts_b])
                dve(lambda e: e.tensor_copy(out=bcum[:, 0, :], in_=base_t[:]), r=[base_b], w=[bcum_b])
                for t in range(1, NTl):
                    dve(lambda e: e.tensor_tensor(out=bcum[:, t, :], in0=bcum[:, t - 1, :], in1=cnts[:, t - 1, :], op=ALU.add), r=[bcum_b, cnts_b], w=[bcum_b])
                dve(lambda e: e.tensor_tensor(out=base_t[:], in0=bcum[:, NTl - 1, :], in1=cnts[:, NTl - 1, :], op=ALU.add), r=[bcum_b, cnts_b], w=[base_b])
                dve(lambda e: e.tensor_tensor(out=rnk[:], in0=rnk[:], in1=bcum[:], op=ALU.add), r=[rnk_b, bcum_b], w=[rnk_b])
                dve(lambda e: e.tensor_scalar(out=okv[:], in0=rnk[:], scalar1=float(CAP), scalar2=None, op0=ALU.is_lt), r=[rnk_b], w=[okv_b])
                dve(lambda e: e.tensor_tensor(out=rnk[:], in0=rnk[:], in1=ec_t[:].unsqueeze(1).to_broadcast([128, NTl, 32]), op=ALU.add), r=[rnk_b, ec_b], w=[rnk_b])
                dve(lambda e: e.scalar_tensor_tensor(out=rnk[:], in0=rnk[:], scalar=float(-NSLOT), in1=okv[:], op0=ALU.add, op1=ALU.mult), r=[rnk_b, okv_b], w=[rnk_b])
                for q in range(2):
                    dve(lambda e: e.tensor_tensor(out=o32[q][0][:], in0=o32[q][0][:], in1=rnk[:], op=ALU.mult), r=[o32[q][1], rnk_b], w=[o32[q][1]])
                    dve(lambda e: e.tensor_reduce(out=slf[:, :, q], in_=o32[q][0][:], axis=AX.X, op=ALU.add), r=[o32[q][1]], w=[slf_b])
                dve(lambda e: e.tensor_scalar(out=slf[:], in0=slf[:], scalar1=float(NSLOT), scalar2=None, op0=ALU.add), r=[slf_b], w=[slf_b])
                dve(lambda e: e.tensor_copy(out=sview, in_=slf[:]), r=[slf_b], w=[rt_b])
                dve(lambda e: e.tensor_copy(out=sidxA[:].rearrange("p (t q) -> p t q", q=2), in_=slf[:]), r=[slf_b], w=[sidxA_b])
                for t in range(NTl):
                    hi = t % 4
                    r0 = s * S + t * 128
                    k.dma("sp", lambda e: e.dma_start(out=hrow[hi][0][:], in_=h3_scr.ap()[r0:r0 + 128, :]), writes=[hrow[hi][1]])
                    for q in range(2):
                        k.dma("pool", lambda e: e.indirect_dma_start(out=xs_scr.ap(), out_offset=bass.IndirectOffsetOnAxis(ap=sidxA[:, 2 * t + q:2 * t + q + 1], axis=0),
                                                                   in_=hrow[hi][0][:, :], in_offset=None),
                              reads=[hrow[hi][1], sidxA_b], pre_nop=True)
            k.barrier()
    if dbg:
        k.dma("sp", lambda e: e.dma_start(out=dbg_d["d_rt"].ap()[:, 0:NTT * 2], in_=gate_f[:]), reads=[rt_b])
        k.barrier()
    if stage <= 4:
        k.finish("sp")
        return nc, dbg_d

    NST = CAP // 128
    with ExitStack() as sb:
        w1b = [salloc(sb, "w1b%d" % i, [128, 8, 512], BF16) for i in range(2)]
        w3b = [salloc(sb, "w3b%d" % i, [128, 8, 512], BF16) for i in range(2)]
        w2b = [salloc(sb, "w2b%d" % i, [128, 4, D], BF16) for i in range(2)]
        w1_b = [Buf("w1b%d" % i) for i in range(2)]
        w3_b = [Buf("w3b%d" % i) for i in range(2)]
        w2_b = [Buf("w2b%d" % i) for i in range(2)]
        xrow = [salloc(sb, "xrow%d" % i, [128, D], BF16) for i in range(3)]
        xrow_b = [Buf("xrow%d" % i) for i in range(3)]
        xsT = [salloc(sb, "xsT%d" % i, [128, 8, CAP], BF16) for i in range(2)]
        xsT_b = [Buf("xsT%d" % i) for i in range(2)]
        HT = [salloc(sb, "HT%d" % i, [128, 4, CAP], BF16) for i in range(2)]
        HT_b = [Buf("HT%d" % i) for i in range(2)]
        tf = [salloc(sb, "etf%d" % i, [128, 512], F32) for i in range(3)]
        tf_b = [Buf("etf%d" % i) for i in range(3)]
        yt = [salloc(sb, "yt%d" % i, [128, D], BF16) for i in range(3)]
        yt_b = [Buf("yt%d" % i) for i in range(3)]

        def load_w(e_):
            wb = e_ % 2
            k.dma("pool", lambda e: e.dma_start(out=w1b[wb][:], in_=din["w1"].ap()[e_].rearrange("(c p) f -> p c f", p=128)), writes=[w1_b[wb]])
            k.dma("pool", lambda e: e.dma_start(out=w3b[wb][:], in_=din["w3"].ap()[e_].rearrange("(c p) f -> p c f", p=128)), writes=[w3_b[wb]])
            k.dma("pool", lambda e: e.dma_start(out=w2b[wb][:], in_=din["w2"].ap()[e_].rearrange("(c p) d -> p c d", p=128)), writes=[w2_b[wb]])

        cregs = [None, None]
        load_w(0)
        cnt_ = {"rx": 0, "ry": 0, "rt3": 0}
        NE = NEXP if stage > 5 or stage == 99 else lim.get("nexp", NEXP)

        def b_front(e_):
            wb = e_ % 2
            for st_ in range(NST):
                xi = cnt_["rx"] % 3
                cnt_["rx"] += 1
                r0 = e_ * CAP + st_ * 128
                k.dma("sp", lambda e: e.dma_start(out=xrow[xi][:], in_=xs_scr.ap()[r0:r0 + 128, :]), writes=[xrow_b[xi]])
                bank = 6 + st_ % 2

                def body():
                    for c in range(8):
                        pe(lambda e: e.transpose(psbf[bank][:, c * 128:(c + 1) * 128], xrow[xi][:, c * 128:(c + 1) * 128], ident_b[:]),
                           r=[xrow_b[xi], ident_b_b], w=[psb[bank]], inc=(c == 7))
                body()
                act(lambda e: e.activation(out=xsT[wb][:, :, st_ * 128:(st_ + 1) * 128], in_=psbf[bank][:, :].rearrange("p (c t) -> p c t", c=8), func=AF.Copy),
                    r=[psb[bank]], w=[xsT_b[wb]])

        def b_stage1(e_):
            wb = e_ % 2
            for (s0, n) in ((0, 512), (512, CAP - 512)):
                for f in range(4):
                    b1, b3 = (0, 1) if f % 2 == 0 else (2, 3)

                    def body():
                        for c in range(8):
                            pe(lambda e: e.matmul(ps[b1][:, 0:n], lhsT=w1b[wb][:, c, f * 128:(f + 1) * 128], rhs=xsT[wb][:, c, s0:s0 + n], start=(c == 0), stop=(c == 7)),
                               r=[w1_b[wb], xsT_b[wb]], w=[psb[b1]], inc=(c == 7))
                        for c in range(8):
                            pe(lambda e: e.matmul(ps[b3][:, 0:n], lhsT=w3b[wb][:, c, f * 128:(f + 1) * 128], rhs=xsT[wb][:, c, s0:s0 + n], start=(c == 0), stop=(c == 7)),
                               r=[w3_b[wb], xsT_b[wb]], w=[psb[b3]], inc=(c == 7))
                    if s0 == 0:
                        body()
                    else:
                        k.pe_cond(cregs[wb], s0 + 1, 2, body)
                    ti = cnt_["rt3"] % 3
                    cnt_["rt3"] += 1
                    act(lambda e: e.activation(out=tf[ti][:, 0:n], in_=ps[b1][:, 0:n], func=AF.Tanh, scale=0.5), r=[psb[b1]], w=[tf_b[ti]])
                    dve(lambda e: e.scalar_tensor_tensor(out=tf[ti][:, 0:n], in0=tf[ti][:, 0:n], scalar=1.0, in1=ps[b1][:, 0:n], op0=ALU.add, op1=ALU.mult),
                        r=[psb[b1], tf_b[ti]], w=[tf_b[ti]])
                    dve(lambda e: e.tensor_tensor(out=HT[wb][:, f, s0:s0 + n], in0=ps[b3][:, 0:n], in1=tf[ti][:, 0:n], op=ALU.mult), r=[psb[b3], tf_b[ti]], w=[HT_b[wb]])

        def b_stage2(e_):
            wb = e_ % 2
            for st_ in range(NST):
                yi = cnt_["ry"] % 3
                cnt_["ry"] += 1
                for half in range(2):
                    bank = 4 + half

                    def body():
                        for f in range(4):
                            pe(lambda e: e.matmul(ps[bank][:], lhsT=HT[wb][:, f, st_ * 128:(st_ + 1) * 128], rhs=w2b[wb][:, f, half * 512:(half + 1) * 512], start=(f == 0), stop=(f == 3)),
                               r=[HT_b[wb], w2_b[wb]], w=[psb[bank]], inc=(f == 3))
                    if st_ == 0:
                        body()
                    else:
                        k.pe_cond(cregs[wb], st_ * 128 + 1, 1, body)
                    act(lambda e: e.activation(out=yt[yi][:, half * 512:(half + 1) * 512], in_=ps[bank][:], func=AF.Copy, scale=0.5), r=[psb[bank]], w=[yt_b[yi]])
                r0 = e_ * CAP + st_ * 128
                k.dma("pool", lambda e: e.dma_start(out=ys_scr.ap()[r0:r0 + 128, :], in_=yt[yi][:]), reads=[yt_b[yi]])

        b_front(0)
        for e_ in range(NE):
            if e_ + 1 < NEXP:
                load_w(e_ + 1)
            b_stage1(e_)
            if e_ + 1 < NE:
                b_front(e_ + 1)
            b_stage2(e_)
        k.barrier()

    with ExitStack() as sc_:
        RC = 4
        x2t = [salloc(sc_, "x2t%d" % i, [128, D], F32) for i in range(RC)]
        x2t_b = [Buf("x2t%d" % i) for i in range(RC)]
        yg = [[salloc(sc_, "yg%d_%d" % (i, q), [128, D], BF16) for q in range(2)] for i in range(RC)]
        yg_b = [[Buf("yg%d_%d" % (i, q)) for q in range(2)] for i in range(RC)]
        gi = [[salloc(sc_, "gi%d_%d" % (i, q), [128, 1], I32) for q in range(2)] for i in range(RC)]
        gi_b = [[Buf("gi%d_%d" % (i, q)) for q in range(2)] for i in range(RC)]
        x3 = [salloc(sc_, "x3_%d" % i, [128, D], F32) for i in range(2)]
        x3_b = [Buf("x3_%d" % i) for i in range(2)]
        jk = [salloc(sc_, "jk%d" % i, [128, D], BF16) for i in range(2)]
        jk_b = [Buf("jk%d" % i) for i in range(2)]
        st3 = [salloc(sc_, "st3_%d" % i, [128, 4], F32) for i in range(2)]
        st3_b = [Buf("st3_%d" % i) for i in range(2)]
        ot = [salloc(sc_, "ot%d" % i, [128, D], F32) for i in range(2)]
        ot_b = [Buf("ot%d" % i) for i in range(2)]

        def c_front(tt):
            i = tt % RC
            r0 = tt * 128
            k.dma("sp", lambda e: e.dma_start(out=x2t[i][:], in_=x2_scr.ap()[r0:r0 + 128, :]), writes=[x2t_b[i]])
            for q in range(2):
                dve(lambda e: e.tensor_copy(out=gi[i][q][:, :], in_=slot_i[:, 2 * tt + q:2 * tt + q + 1]), r=[rt_b], w=[gi_b[i][q]])
                k.dma("pool", lambda e: e.indirect_dma_start(out=yg[i][q][:, :], out_offset=None, in_=ys_scr.ap(),
                                                           in_offset=bass.IndirectOffsetOnAxis(ap=gi[i][q][:, :], axis=0)),
                      reads=[gi_b[i][q]], writes=[yg_b[i][q]], pre_nop=True)

        def c_back(tt):
            i = tt % RC
            j = tt % 2
            r0 = tt * 128
            dve(lambda e: e.scalar_tensor_tensor(out=x3[j][:], in0=yg[i][0][:], scalar=gate_f[:, 2 * tt:2 * tt + 1], in1=x2t[i][:], op0=ALU.mult, op1=ALU.add),
                r=[yg_b[i][0], rt_b, x2t_b[i]], w=[x3_b[j]])
            dve(lambda e: e.scalar_tensor_tensor(out=x3[j][:], in0=yg[i][1][:], scalar=gate_f[:, 2 * tt + 1:2 * tt + 2], in1=x3[j][:], op0=ALU.mult, op1=ALU.add),
                r=[yg_b[i][1], rt_b, x3_b[j]], w=[x3_b[j]])
            act(lambda e: e.activation(out=jk[j][:], in_=x3[j][:], func=AF.Square, accum_out=st3[j][:, 0:1]), r=[x3_b[j]], w=[jk_b[j], st3_b[j]])
            rstd_ops(st3[j], st3_b[j], 1.0 / D, EPS)
            dve(lambda e: e.scalar_tensor_tensor(out=ot[j][:], in0=x3[j][:], scalar=st3[j][:, 2:3], in1=gfin[:], op0=ALU.mult, op1=ALU.mult),
                r=[x3_b[j], st3_b[j], gfin_b], w=[ot_b[j]])
            k.dma("sp", lambda e: e.dma_start(out=out_d.ap()[r0:r0 + 128, :], in_=ot[j][:]), reads=[ot_b[j]])

        PFC = 2
        for tt in range(min(PFC, NTT)):
            c_front(tt)
        for tt in range(NTT):
            if tt + PFC < NTT:
                c_front(tt + PFC)
            c_back(tt)
        k.barrier()

    k.finish("sp")
    return nc, dbg_d


def make_in_map(inputs, core, nseq):
    m = {}
    b0 = core * nseq
    m["x"] = np.ascontiguousarray(np.asarray(inputs["x"])[b0:b0 + nseq].reshape(nseq * S, D))
    m["mem"] = np.ascontiguousarray(np.asarray(inputs["mem"])[b0:b0 + nseq].reshape(nseq * MEM, D))
    m["positions"] = np.ascontiguousarray(np.asarray(inputs["positions"]).astype(np.int32))
    for k_, shp in WEIGHT_SHAPES.items():
        a = np.asarray(inputs[k_])
        if k_ != "final_norm_g":
            a = a[0]
        m[k_] = np.ascontiguousarray(a.reshape(shp).astype(np.float32))
    m.update(host_consts())
    return m


_NC_CACHE = {}


def kernel(**inputs):
    n_cores = 8
    nseq = 2
    if "nc" not in _NC_CACHE:
        _NC_CACHE["nc"] = build(nseq=nseq)[0]
    nc = _NC_CACHE["nc"]
    consts = host_consts()
    shared = None
    in_maps = []
    for c in range(n_cores):
        m = make_in_map(inputs, c, nseq) if shared is None else dict(shared)
        if shared is None:
            shared = {k_: v for k_, v in m.items() if k_ not in ("x", "mem")}
        else:
            b0 = c * nseq
            m["x"] = np.ascontiguousarray(np.asarray(inputs["x"])[b0:b0 + nseq].reshape(nseq * S, D))
            m["mem"] = np.ascontiguousarray(np.asarray(inputs["mem"])[b0:b0 + nseq].reshape(nseq * MEM, D))
        in_maps.append(m)
    res = run_bass_kernel_spmd(nc, in_maps, core_ids=list(range(n_cores)))
    outs = [np.asarray(r["out"]).reshape(nseq, S, D) for r in res.results]
    return np.concatenate(outs, axis=0).astype(np.float32)
```

```python
import numpy as np
import ml_dtypes
import concourse.bass as bass
import concourse.mybir as mybir
from concourse.bass_utils import run_bass_kernel_spmd

F32 = mybir.dt.float32
BF16 = mybir.dt.bfloat16
I32 = mybir.dt.int32
AF = mybir.ActivationFunctionType
ALU = mybir.AluOpType
AX = mybir.AxisListType

S = 4096
D = 1024
NT = S // 128
NCH = S // 512
MEM = 256
NEXP = 32
CAP = 1024
NSLOT = NEXP * CAP
EPS = 1e-6
LN_EPS = 1e-5
SEM_LIMIT = 30000


class Buf:
    __slots__ = ("name", "w", "r", "excl")

    def __init__(self, name, excl=False):
        self.name = name
        self.w = None
        self.r = {}
        self.excl = excl


class K:
    def __init__(self, nc):
        self.nc = nc
        self.eng = {"pe": nc.tensor, "act": nc.scalar, "dve": nc.vector, "pool": nc.gpsimd, "sp": nc.sync}
        self.sem = {}
        self.cnt = {}
        self.own = {e: set() for e in self.eng}
        self.seen = {e: {} for e in self.eng}
        self.nsem = 0
        self.allsems = []
        for e in self.eng:
            self._newsem(e)
        self.dq = {}
        for q, n in (("sp", 12), ("pool", 12), ("poolz", 6)):
            sems = [self._mksem("d_%s%d" % (q, i)) for i in range(n)]
            self.dq[q] = {"sems": sems, "cnt": [0] * len(sems), "rr": 0}
        self.det_ev = {}
        self.pending = {e: False for e in self.eng}
        self.last_ev = {}

    def _mksem(self, name):
        s = self.nc.alloc_semaphore(name)
        self.nsem += 1
        self.allsems.append(s)
        return s

    def _newsem(self, e):
        self.sem[e] = self._mksem("s_%s%d" % (e, self.nsem))
        self.own[e].add(self.sem[e])
        self.cnt[e] = 0

    def wait(self, e, evs):
        need = {}
        for ev in evs:
            if ev is None:
                continue
            sem, val = ev
            if e == "pe" and sem is self.sem["pe"]:
                continue
            if sem is self.sem[e] and val > self.cnt[e]:
                continue
            if self.seen[e].get(sem, 0) < val and need.get(sem, 0) < val:
                need[sem] = val
        for sem, val in need.items():
            self.eng[e].wait_ge(sem, val)
            self.seen[e][sem] = val

    def _deps(self, reads, writes, e=None):
        evs = []
        for b in reads:
            evs.append(b.w)
            if b.excl:
                evs.extend(it for it in b.r.items() if it[0] not in self.own.get(e, ()))
        for b in writes:
            evs.append(b.w)
            evs.extend(b.r.items())
        return evs

    def _mark(self, ev, reads, writes):
        sem, val = ev
        for b in reads:
            if b.r.get(sem, 0) < val:
                b.r[sem] = val
        for b in writes:
            b.w = ev
            b.r = {}
        self.last_ev[sem] = max(self.last_ev.get(sem, 0), val)

    def op(self, e, fn, reads=(), writes=(), inc=True):
        self.wait(e, self._deps(reads, writes, e))
        ins = fn(self.eng[e])
        if inc:
            if self.cnt[e] >= SEM_LIMIT and not self.pending[e]:
                self._newsem(e)
            self.cnt[e] += 1
            ins.then_inc(self.sem[e], 1)
            ev = (self.sem[e], self.cnt[e])
            self.pending[e] = False
        else:
            ev = (self.sem[e], self.cnt[e] + 1)
            self.pending[e] = True
        self._mark(ev, reads, writes)
        return ev

    def dma(self, q, fn, reads=(), writes=(), pre_nop=False):
        dq = self.dq[q]
        i = dq["rr"]
        dq["rr"] = (i + 1) % len(dq["sems"])
        sem = dq["sems"][i]
        evs = self._deps(reads, writes)
        evs.append((sem, 16 * dq["cnt"][i]) if dq["cnt"][i] else None)
        qe = "pool" if q == "poolz" else q
        self.wait(qe, evs)
        if pre_nop:
            self.eng[qe].nop()
        ins = fn(self.eng[qe])
        dq["cnt"][i] += 1
        ins.then_inc(sem, 16)
        ev = (sem, 16 * dq["cnt"][i])
        self._mark(ev, reads, writes)
        if q == "poolz":
            self.det_ev[sem] = ev[1]
            if self.last_ev.get(sem) == ev[1]:
                del self.last_ev[sem]
        return ev

    def join_detached(self):
        for sem, val in self.det_ev.items():
            self.last_ev[sem] = max(self.last_ev.get(sem, 0), val)
        self.det_ev = {}

    def pe_cond(self, reg, thresh, n_inc, body):
        body()

    def barrier(self):
        evs = list(self.last_ev.items())
        for e in self.eng:
            self.wait(e, evs)

    def finish(self, e="sp"):
        self.wait(e, list(self.last_ev.items()))


def _bf(a):
    return np.ascontiguousarray(a.astype(np.float32))


def host_consts():
    c = {}
    c["c_ident"] = np.eye(128, dtype=np.float32)
    rt = np.zeros((128, 128), np.float32)
    for h in range(2):
        for i in range(32):
            rt[h * 64 + i + 32, h * 64 + i] = -1.0
            rt[h * 64 + i, h * 64 + i + 32] = 1.0
    c["c_rot"] = rt
    jj = np.arange(128)[:, None]
    ii = np.arange(128)[None, :]
    mlo = (ii >= jj).astype(np.float32)
    mhi = (ii <= jj).astype(np.float32)
    c["c_masklh"] = np.concatenate([mlo, mhi], axis=1)
    c["c_maskf"] = np.concatenate([mhi[64:128], np.zeros((64, 128), np.float32)], axis=0)
    c["c_tri"] = (jj < ii).astype(np.float32)
    half = 32
    inv = (1.0 / (np.float32(10000.0) ** (np.arange(half, dtype=np.float32) * np.float32(2.0 / 64)))).astype(np.float32)
    c["c_inv"] = np.tile(inv, 4).reshape(128, 1).astype(np.float32)
    c["c_ec"] = np.tile((np.arange(NEXP, dtype=np.float32) * CAP)[None, :], (128, 1))
    es = np.zeros((128, 64), np.float32)
    es[64, :] = 1.0
    c["c_esel"] = es
    return c


CONST_SHAPES = {"c_ident": [128, 128], "c_rot": [128, 128], "c_masklh": [128, 256], "c_maskf": [128, 128],
                "c_tri": [128, 128], "c_inv": [128, 1], "c_ec": [128, NEXP], "c_esel": [128, 64]}

WEIGHT_SHAPES = {
    "mix_norm_g": [D], "w_in": [D, 2816], "conv_dw_w": [31, 256], "conv_dw_b": [256], "conv_ln_g": [256],
    "conv_ln_b": [256], "conv_out_g": [256], "attn_out_g": [768], "w_out": [D, D], "xattn_norm_g": [D],
    "mem_norm_g": [D], "w_xq": [D, D], "w_xk": [D, D], "w_xv": [D, D], "w_xo": [D, D], "moe_norm_g": [D],
    "w_group": [D, 4], "b_group": [4], "w_router": [D, 32], "b_router": [32],
    "w1": [NEXP, D, 512], "w3": [NEXP, D, 512], "w2": [NEXP, 512, D], "final_norm_g": [D],
}


from contextlib import ExitStack

PI_SAFE = 3.141592
TWO_PI = 6.283185307179586
C1 = 6.28125
C2 = TWO_PI - C1


def build(nseq=2, stage=99, dbg=False, lim=None):
    lim = lim or {}
    nc = bass.Bass("TRN2", target_bir_lowering=False)
    T = nseq * S
    din = {}
    din["x"] = nc.dram_tensor("x", [T, D], F32, kind="ExternalInput")
    din["mem"] = nc.dram_tensor("mem", [nseq * MEM, D], F32, kind="ExternalInput")
    din["positions"] = nc.dram_tensor("positions", [S], I32, kind="ExternalInput")
    for k_, shp in WEIGHT_SHAPES.items():
        din[k_] = nc.dram_tensor(k_, shp, F32, kind="ExternalInput")
    for k_, shp in CONST_SHAPES.items():
        din[k_] = nc.dram_tensor(k_, shp, F32, kind="ExternalInput")
    out_d = nc.dram_tensor("out", [T, D], F32, kind="ExternalOutput")
    kscr = "ExternalOutput" if dbg else "Internal"
    o_scr = nc.dram_tensor("o_scr", [nseq, 768, S], BF16, kind=kscr)
    x2_scr = nc.dram_tensor("x2_scr", [T, D], F32, kind=kscr)
    tab_scr = nc.dram_tensor("tab_scr", [2, 128, S], BF16)
    h3_scr = nc.dram_tensor("h3_scr", [T, D], BF16)
    xs_scr = nc.dram_tensor("xs_scr", [NSLOT + 128, D], BF16)
    ys_scr = nc.dram_tensor("ys_scr", [NSLOT + 128, D], BF16)
    dbg_d = {}
    if dbg:
        dbg_d["d_hT"] = nc.dram_tensor("d_hT", [nseq, 128, 8, S], BF16, kind="ExternalOutput")
        dbg_d["d_zT"] = nc.dram_tensor("d_zT", [nseq, 128, 2, S], BF16, kind="ExternalOutput")
        dbg_d["d_rt"] = nc.dram_tensor("d_rt", [128, nseq * NT * 4], F32, kind="ExternalOutput")

    k = K(nc)
    A = nc.alloc_sbuf_tensor

    def pe(fn, r=(), w=(), inc=True):
        return k.op("pe", fn, r, w, inc)

    def act(fn, r=(), w=()):
        return k.op("act", fn, r, w)

    def dve(fn, r=(), w=()):
        return k.op("dve", fn, r, w)

    def pool(fn, r=(), w=()):
        return k.op("pool", fn, r, w)

    uid = [0]

    def salloc(stack, name, shape, dtype):
        uid[0] += 1
        return stack.enter_context(nc.sbuf_tensor("%s_u%d" % (name, uid[0]), shape, dtype))

    ps2 = [nc.alloc_psum_tensor("ps2_%d" % i, [128, 1024], F32) for i in range(4)]
    ps = []
    for i in range(4):
        ps.append(ps2[i][:, 0:512])
        ps.append(ps2[i][:, 512:1024])
    psbf = [p.bitcast(BF16) for p in ps]
    psb = [Buf("ps%d" % i, excl=True) for i in range(8)]

    def load_const(name, src, shape, dtype):
        t = A(name, shape, dtype)
        b = Buf(name)
        q = "sp" if dtype == F32 else "pool"
        k.dma(q, lambda e: e.dma_start(out=t[:], in_=src), writes=[b])
        return t, b

    ident_f, ident_f_b = load_const("ident_f", din["c_ident"].ap(), [128, 128], F32)
    ident_b, ident_b_b = load_const("ident_b", din["c_ident"].ap(), [128, 128], BF16)
    rot_b, rot_b_b = load_const("rot_b", din["c_rot"].ap(), [128, 128], BF16)
    masklh, masklh_b = load_const("masklh", din["c_masklh"].ap(), [128, 256], BF16)
    maskf, maskf_b = load_const("maskf", din["c_maskf"].ap(), [128, 128], BF16)
    tri_b, tri_b_b = load_const("tri_b", din["c_tri"].ap(), [128, 128], BF16)
    inv_t, inv_b = load_const("inv_t", din["c_inv"].ap(), [128, 1], F32)
    ec_t, ec_b = load_const("ec_t", din["c_ec"].ap(), [128, NEXP], F32)
    esel, esel_b = load_const("esel", din["c_esel"].ap(), [128, 64], F32)

    ones_b = A("ones_b", [128, 128], BF16)
    ones_b_b = Buf("ones_b")
    dve(lambda e: e.memset(ones_b[:], 1.0), w=[ones_b_b])
    ones_f = A("ones_f", [128, 128], F32)
    ones_f_b = Buf("ones_f")
    dve(lambda e: e.memset(ones_f[:], 1.0), w=[ones_f_b])

    def gvec(name, src_ap, nchunk):
        t = A(name, [128, nchunk], F32)
        b = Buf(name)
        with nc.allow_non_contiguous_dma(reason="tiny gain vector"):
            k.dma("sp", lambda e: e.dma_start(out=t[:], in_=src_ap.rearrange("(c p) -> p c", p=128)), writes=[b])
        return t, b

    g_mix, g_mix_b = gvec("g_mix", din["mix_norm_g"].ap(), 8)
    g_xn, g_xn_b = gvec("g_xn", din["xattn_norm_g"].ap(), 8)
    g_mem, g_mem_b = gvec("g_mem", din["mem_norm_g"].ap(), 8)
    g_mx = A("g_mx", [128, 8], F32)
    g_mx_b = Buf("g_mx")
    with nc.allow_non_contiguous_dma(reason="tiny gain vector"):
        k.dma("sp", lambda e: e.dma_start(out=g_mx[:, 0:2], in_=din["conv_out_g"].ap().rearrange("(c p) -> p c", p=128)), writes=[g_mx_b])
        k.dma("sp", lambda e: e.dma_start(out=g_mx[:, 2:8], in_=din["attn_out_g"].ap().rearrange("(c p) -> p c", p=128)), writes=[g_mx_b])
    cv_b, cv_b_b = gvec("cv_b", din["conv_dw_b"].ap(), 2)
    ln_g, ln_g_b = gvec("ln_g", din["conv_ln_g"].ap(), 2)
    ln_b, ln_b_b = gvec("ln_b", din["conv_ln_b"].ap(), 2)
    wdw = A("wdw", [128, 2, 31], F32)
    wdw_b = Buf("wdw")
    with nc.allow_non_contiguous_dma(reason="tiny conv weights"):
        for c2 in range(2):
            k.dma("sp", lambda e: e.dma_start(out=wdw[:, c2, :], in_=din["conv_dw_w"].ap()[:, c2 * 128:(c2 + 1) * 128].rearrange("k p -> p k")), writes=[wdw_b])

    def bcast_rows(name, src_ap, n, dst=None, off=0):
        if dst is None:
            t = A(name, [128, n], F32)
            b = Buf(name)
            k.dma("sp", lambda e: e.dma_start(out=t[:], in_=src_ap.partition_broadcast(128)), writes=[b])
            return t, b
        k.dma("sp", lambda e: e.dma_start(out=dst[0][:, off:off + n], in_=src_ap.partition_broadcast(128)), writes=[dst[1]])

    gfin, gfin_b = bcast_rows("gfin", din["final_norm_g"].ap(), D)
    gmoe, gmoe_b = bcast_rows("gmoe", din["moe_norm_g"].ap(), D)
    brt = A("brt", [128, 36], F32)
    brt_b = Buf("brt")
    bcast_rows(None, din["b_group"].ap(), 4, dst=(brt, brt_b), off=0)
    bcast_rows(None, din["b_router"].ap(), 32, dst=(brt, brt_b), off=4)
    wr = A("wr", [128, 8, 36], F32)
    wr_b = Buf("wr")
    with nc.allow_non_contiguous_dma(reason="small router weights"):
        k.dma("sp", lambda e: e.dma_start(out=wr[:, :, 0:4], in_=din["w_group"].ap().rearrange("(c p) n -> p c n", p=128)), writes=[wr_b])
        k.dma("sp", lambda e: e.dma_start(out=wr[:, :, 4:36], in_=din["w_router"].ap().rearrange("(c p) n -> p c n", p=128)), writes=[wr_b])

    wrb = A("wrb", [128, 8, 36], BF16)
    wrb_b = Buf("wrb")
    k.barrier()
    dve(lambda e: e.tensor_copy(out=wrb[:], in_=wr[:]), r=[wr_b], w=[wrb_b])
    NTT = nseq * NT
    slot_i = A("slot_i", [128, NTT * 2], I32)
    gate_f = A("gate_f", [128, NTT * 2], F32)
    rt_b = Buf("rt")
    base_t = A("base_t", [128, NEXP], F32)
    base_b = Buf("base")
    dve(lambda e: e.memset(base_t[:], 0.0), w=[base_b])
    zT = A("zT", [128, 2, S], BF16)
    zT_b = [Buf("zT%d" % i) for i in range(NCH)]

    zt = A("zt", [128, 2, D], BF16)
    zt_b = Buf("zt")
    dve(lambda e: e.memset(zt[:], 0.0), w=[zt_b])
    with ExitStack() as ss:
        pos_i = salloc(ss, "pos_i", [128, S], I32)
        ang = salloc(ss, "ang", [128, S], F32)
        kf = salloc(ss, "kf", [128, S], F32)
        ki = salloc(ss, "ki", [128, S], I32)
        rr = salloc(ss, "rr", [128, S], F32)
        r2 = salloc(ss, "r2", [128, S], F32)
        tb = salloc(ss, "tb", [128, S], BF16)
        tb2 = salloc(ss, "tb2", [128, S], BF16)
        b_pos, b_ang, b_kf, b_ki, b_rr, b_r2, b_tb, b_tb2 = [Buf(n) for n in "pos ang kf ki rr r2 tb tb2".split()]
        k.dma("sp", lambda e: e.dma_start(out=pos_i[:], in_=din["positions"].ap().partition_broadcast(128)), writes=[b_pos])
        dve(lambda e: e.tensor_copy(out=ang[:], in_=pos_i[:]), r=[b_pos], w=[b_ang])
        dve(lambda e: e.tensor_scalar(out=ang[:], in0=ang[:], scalar1=inv_t[:, 0:1], scalar2=None, op0=ALU.mult), r=[b_ang, inv_b], w=[b_ang])
        dve(lambda e: e.tensor_scalar(out=ki[:], in0=ang[:], scalar1=float(1.0 / TWO_PI), scalar2=None, op0=ALU.mult), r=[b_ang], w=[b_ki])
        dve(lambda e: e.tensor_copy(out=kf[:], in_=ki[:]), r=[b_ki], w=[b_kf])
        dve(lambda e: e.scalar_tensor_tensor(out=rr[:], in0=kf[:], scalar=-C1, in1=ang[:], op0=ALU.mult, op1=ALU.add), r=[b_kf, b_ang], w=[b_rr])
        dve(lambda e: e.scalar_tensor_tensor(out=rr[:], in0=kf[:], scalar=-C2, in1=rr[:], op0=ALU.mult, op1=ALU.add), r=[b_kf, b_rr], w=[b_rr])
        dve(lambda e: e.tensor_scalar(out=r2[:], in0=rr[:], scalar1=float(np.pi / 2), scalar2=None, op0=ALU.add), r=[b_rr], w=[b_r2])
        dve(lambda e: e.tensor_scalar(out=kf[:], in0=r2[:], scalar1=float(np.pi), scalar2=None, op0=ALU.is_gt), r=[b_r2], w=[b_kf])
        dve(lambda e: e.scalar_tensor_tensor(out=r2[:], in0=kf[:], scalar=-TWO_PI, in1=r2[:], op0=ALU.mult, op1=ALU.add), r=[b_kf, b_r2], w=[b_r2])
        dve(lambda e: e.tensor_scalar(out=rr[:], in0=rr[:], scalar1=PI_SAFE, scalar2=-PI_SAFE, op0=ALU.min, op1=ALU.max), r=[b_rr], w=[b_rr])
        dve(lambda e: e.tensor_scalar(out=r2[:], in0=r2[:], scalar1=PI_SAFE, scalar2=-PI_SAFE, op0=ALU.min, op1=ALU.max), r=[b_r2], w=[b_r2])
        act(lambda e: e.activation(out=tb[:], in_=r2[:], func=AF.Sin), r=[b_r2], w=[b_tb])
        act(lambda e: e.activation(out=tb2[:], in_=rr[:], func=AF.Sin), r=[b_rr], w=[b_tb2])
        k.dma("sp", lambda e: e.dma_start(out=tab_scr.ap()[0], in_=tb[:]), reads=[b_tb])
        k.dma("sp", lambda e: e.dma_start(out=tab_scr.ap()[1], in_=tb2[:]), reads=[b_tb2])
        k.barrier()

    xs_v = xs_scr.ap().rearrange("(n p) d -> p n d", p=128)
    zf_evs = []
    for i in range((NSLOT + 128) // 128 // 2 + 0):
        zf_evs.append(k.dma("poolz", lambda e: e.dma_start(out=xs_v[:, i * 2:(i + 1) * 2, :], in_=zt[:]), reads=[zt_b]))
    if (NSLOT + 128) // 128 % 2:
        n_ = (NSLOT + 128) // 128 - 1
        zf_evs.append(k.dma("poolz", lambda e: e.dma_start(out=xs_v[:, n_:n_ + 1, :], in_=zt[:, 0:1, :]), reads=[zt_b]))
    zf_evs.append(k.dma("poolz", lambda e: e.dma_start(out=ys_scr.ap()[NSLOT:NSLOT + 128, :], in_=zt[:, 0, :]), reads=[zt_b]))
    x_d = din["x"].ap()
    win = din["w_in"].ap()

    def wslice(ap2d, c0, n):
        return ap2d[:, c0:c0 + n].rearrange("(c p) n -> p c n", p=128)

    def rstd_ops(stat, stat_b, inv_n, eps):
        act(lambda e: e.activation(out=stat[:, 1:2], in_=stat[:, 0:1], func=AF.Ln, scale=inv_n, bias=eps_ap(eps)), r=[stat_b, epsb], w=[stat_b])
        act(lambda e: e.activation(out=stat[:, 2:3], in_=stat[:, 1:2], func=AF.Exp, scale=-0.5), r=[stat_b], w=[stat_b])

    epst = A("epst", [128, 4], F32)
    epsb = Buf("epst")
    dve(lambda e: e.memset(epst[:, 0:1], EPS), w=[epsb])
    dve(lambda e: e.memset(epst[:, 1:2], LN_EPS), w=[epsb])
    dve(lambda e: e.memset(epst[:, 2:3], 1.0), w=[epsb])
    one_ap = epst[:, 2:3]

    def eps_ap(eps):
        return epst[:, 0:1] if eps == EPS else epst[:, 1:2]

    pend_scatter = []
    hz_b = [Buf("hz0"), Buf("hz1")]
    sc_cnt = [0]

    def scatter_step():
        if not pend_scatter:
            return False
        s_, t_ = pend_scatter.pop(0)
        j = sc_cnt[0] % 2
        sc_cnt[0] += 1
        r0 = s_ * S + t_ * 128
        tt = s_ * NT + t_
        k.dma("sp", lambda e: e.dma_start(out=zt[:, j, :], in_=h3_scr.ap()[r0:r0 + 128, :]), reads=[zt_b], writes=[hz_b[j]])
        for q in range(2):
            k.dma("poolz", lambda e: e.indirect_dma_start(out=xs_scr.ap(), out_offset=bass.IndirectOffsetOnAxis(ap=slot_i[:, 2 * tt + q:2 * tt + q + 1], axis=0),
                                                        in_=zt[:, j, :], in_offset=None),
                  reads=[hz_b[j], rt_b], pre_nop=True)
        return True

    for s in range(nseq):
        with ExitStack() as a1:
            hT = salloc(a1, "hT", [128, 8, S], BF16)
            hT_b = [Buf("hT%d" % i) for i in range(NCH)]
            cos_t = salloc(a1, "cos_t", [128, S], BF16)
            sin_t = salloc(a1, "sin_t", [128, S], BF16)
            tab_b = Buf("tab")
            k.dma("sp", lambda e: e.dma_start(out=cos_t[:], in_=tab_scr.ap()[0]), writes=[tab_b])
            k.dma("sp", lambda e: e.dma_start(out=sin_t[:], in_=tab_scr.ap()[1]), writes=[tab_b])
            k.barrier()
            with ExitStack() as sn:
                NR = 6
                xt = [salloc(sn, "xt%d" % i, [128, D], F32) for i in range(NR)]
                xt_b = [Buf("xt%d" % i) for i in range(NR)]
                hb = [salloc(sn, "hb%d" % i, [128, D], BF16) for i in range(NR)]
                hb_b = [Buf("hb%d" % i) for i in range(NR)]
                stt = [salloc(sn, "stt%d" % i, [128, 4], F32) for i in range(NR)]
                stt_b = [Buf("stt%d" % i) for i in range(NR)]

                def n_load(t):
                    r0 = s * S + t * 128
                    k.dma("sp", lambda e: e.dma_start(out=xt[t % NR][:], in_=x_d[r0:r0 + 128, :]), writes=[xt_b[t % NR]])
                    scatter_step()

                def n_gen(t):
                    i = t % NR
                    pb = t % 4
                    act(lambda e: e.activation(out=hb[i][:], in_=xt[i][:], func=AF.Square, accum_out=stt[i][:, 0:1]),
                        r=[xt_b[i]], w=[hb_b[i], stt_b[i]])
                    yield
                    act(lambda e: e.activation(out=stt[i][:, 1:2], in_=stt[i][:, 0:1], func=AF.Ln, scale=1.0 / D, bias=eps_ap(EPS)), r=[stt_b[i], epsb], w=[stt_b[i]])
                    yield
                    act(lambda e: e.activation(out=stt[i][:, 2:3], in_=stt[i][:, 1:2], func=AF.Exp, scale=-0.5), r=[stt_b[i]], w=[stt_b[i]])
                    yield
                    act(lambda e: e.activation(out=hb[i][:], in_=xt[i][:], func=AF.Copy, scale=stt[i][:, 2:3]),
                        r=[xt_b[i], stt_b[i]], w=[hb_b[i]])
                    yield
                    for c in range(8):
                        pe(lambda e: e.transpose(psbf[pb][:, c * 128:(c + 1) * 128], hb[i][:, c * 128:(c + 1) * 128], ident_b[:]),
                           r=[hb_b[i], ident_b_b], w=[psb[pb]], inc=(c == 7))
                    yield
                    dve(lambda e: e.tensor_tensor(out=hT[:, :, t * 128:(t + 1) * 128],
                                                  in0=psbf[pb][:, :].rearrange("p (c t) -> p c t", c=8),
                                                  in1=g_mix[:, :].unsqueeze(2).to_broadcast([128, 8, 128]), op=ALU.mult),
                        r=[psb[pb], g_mix_b], w=[hT_b[t // 4]])

                for t in range(min(NR, NT)):
                    n_load(t)
                nact = []
                nn = 0
                while nn < NT or nact:
                    while len(nact) < 4 and nn < NT:
                        nact.append((n_gen(nn), nn))
                        nn += 1
                    for g_ in list(nact):
                        try:
                            next(g_[0])
                        except StopIteration:
                            nact.remove(g_)
                            if g_[1] + NR < NT:
                                n_load(g_[1] + NR)
                k.barrier()
            if dbg:
                k.dma("sp", lambda e: e.dma_start(out=dbg_d["d_hT"].ap()[s], in_=hT[:]), reads=hT_b)
            if stage <= 1:
                k.barrier()
                continue
            with ExitStack() as sc:
                wconv = salloc(sc, "wconv", [128, 8, 512], BF16)
                wconv_b = Buf("wconv")
                k.dma("pool", lambda e: e.dma_start(out=wconv[:], in_=wslice(win, 0, 512)), writes=[wconv_b])
                cT = salloc(sc, "cT", [128, 2, S + 32], BF16)
                cT_b = Buf("cT")
                diag = salloc(sc, "diag", [128, 2, 31, 128], BF16)
                diag_b = Buf("diag")
                tmpf = [salloc(sc, "tmpf%d" % i, [128, 512], F32) for i in range(10)]
                tmpf_b = [Buf("tmpf%d" % i) for i in range(10)]
                pool(lambda e: e.memset(cT[:, :, 0:15], 0.0), w=[cT_b])
                pool(lambda e: e.memset(cT[:, :, S + 15:S + 32], 0.0), w=[cT_b])
                for c2 in range(2):
                    for kk in range(31):
                        dve(lambda e: e.tensor_scalar(out=diag[:, c2, kk, :], in0=ident_f[:], scalar1=wdw[:, c2, kk:kk + 1], scalar2=None, op0=ALU.mult),
                            r=[ident_f_b, wdw_b], w=[diag_b])
                for ch in range(NCH):
                    cols = slice(ch * 512, (ch + 1) * 512)
                    base = (ch % 2) * 4
                    for j in range(4):
                        for c in range(8):
                            pe(lambda e: e.matmul(ps[base + j][:], lhsT=wconv[:, c, j * 128:(j + 1) * 128], rhs=hT[:, c, cols], start=(c == 0), stop=(c == 7)),
                               r=[wconv_b, hT_b[ch]], w=[psb[base + j]], inc=(c == 7))
                    for c2 in range(2):
                        tf, tfb = tmpf[c2], tmpf_b[c2]
                        act(lambda e: e.activation(out=tf[:], in_=ps[base + 2 + c2][:], func=AF.Exp, scale=-1.0), r=[psb[base + 2 + c2]], w=[tfb])
                        act(lambda e: e.activation(out=tf[:], in_=tf[:], func=AF.Ln, bias=one_ap), r=[tfb, epsb], w=[tfb])
                        act(lambda e: e.activation(out=tf[:], in_=tf[:], func=AF.Exp, scale=-1.0), r=[tfb], w=[tfb])
                        dve(lambda e: e.tensor_tensor(out=cT[:, c2, 15 + ch * 512:15 + (ch + 1) * 512], in0=ps[base + c2][:], in1=tf[:], op=ALU.mult),
                            r=[psb[base + c2], tfb], w=[cT_b])
                for ch in range(NCH):
                    cols = slice(ch * 512, (ch + 1) * 512)
                    base = (ch % 2) * 4
                    yb = [tmpf[0], tmpf[1]]
                    ybb = [tmpf_b[0], tmpf_b[1]]
                    ysq = [tmpf[2], tmpf[3]]
                    ysqb = [tmpf_b[2], tmpf_b[3]]
                    for c2 in range(2):
                        for kk in range(31):
                            pe(lambda e: e.matmul(ps[base + c2][:], lhsT=diag[:, c2, kk, :], rhs=cT[:, c2, ch * 512 + kk:ch * 512 + kk + 512], start=(kk == 0), stop=(kk == 30)),
                               r=[diag_b, cT_b], w=[psb[base + c2]], inc=(kk == 30))
                        act(lambda e: e.activation(out=yb[c2][:], in_=ps[base + c2][:], func=AF.Identity, bias=cv_b[:, c2:c2 + 1]),
                            r=[psb[base + c2], cv_b_b], w=[ybb[c2]])
                        act(lambda e: e.activation(out=ysq[c2][:], in_=yb[c2][:], func=AF.Square), r=[ybb[c2]], w=[ysqb[c2]])
                    for c2 in range(2):
                        pe(lambda e: e.matmul(ps[base + 2][:], lhsT=ones_f[:], rhs=yb[c2][:], start=(c2 == 0), stop=(c2 == 1)),
                           r=[ones_f_b, ybb[c2]], w=[psb[base + 2]], inc=(c2 == 1))
                    for c2 in range(2):
                        pe(lambda e: e.matmul(ps[base + 3][:], lhsT=ones_f[:], rhs=ysq[c2][:], start=(c2 == 0), stop=(c2 == 1)),
                           r=[ones_f_b, ysqb[c2]], w=[psb[base + 3]], inc=(c2 == 1))
                    mean_s, mean_sb = tmpf[4], tmpf_b[4]
                    var_s, var_sb = tmpf[5], tmpf_b[5]
                    act(lambda e: e.activation(out=mean_s[:], in_=ps[base + 2][:], func=AF.Copy, scale=1.0 / 256), r=[psb[base + 2]], w=[mean_sb])
                    dve(lambda e: e.tensor_tensor(out=var_s[:], in0=mean_s[:], in1=mean_s[:], op=ALU.mult), r=[mean_sb], w=[var_sb])
                    dve(lambda e: e.scalar_tensor_tensor(out=var_s[:], in0=ps[base + 3][:], scalar=1.0 / 256, in1=var_s[:], op0=ALU.mult, op1=ALU.subtract),
                        r=[psb[base + 3], var_sb], w=[var_sb])
                    act(lambda e: e.activation(out=var_s[:], in_=var_s[:], func=AF.Ln, bias=eps_ap(LN_EPS)), r=[var_sb, epsb], w=[var_sb])
                    act(lambda e: e.activation(out=var_s[:], in_=var_s[:], func=AF.Exp, scale=-0.5), r=[var_sb], w=[var_sb])
                    for c2 in range(2):
                        dd, ddb = tmpf[6 + c2], tmpf_b[6 + c2]
                        ee, eeb = tmpf[8 + c2], tmpf_b[8 + c2]
                        dve(lambda e: e.tensor_tensor(out=dd[:], in0=yb[c2][:], in1=mean_s[:], op=ALU.subtract), r=[ybb[c2], mean_sb], w=[ddb])
                        dve(lambda e: e.tensor_tensor(out=dd[:], in0=dd[:], in1=var_s[:], op=ALU.mult), r=[ddb, var_sb], w=[ddb])
                        dve(lambda e: e.tensor_scalar(out=dd[:], in0=dd[:], scalar1=ln_g[:, c2:c2 + 1], scalar2=ln_b[:, c2:c2 + 1], op0=ALU.mult, op1=ALU.add),
                            r=[ddb, ln_g_b, ln_b_b], w=[ddb])
                        act(lambda e: e.activation(out=ee[:], in_=dd[:], func=AF.Exp, scale=-1.0), r=[ddb], w=[eeb])
                        act(lambda e: e.activation(out=ee[:], in_=ee[:], func=AF.Ln, bias=one_ap), r=[eeb, epsb], w=[eeb])
                        act(lambda e: e.activation(out=ee[:], in_=ee[:], func=AF.Exp, scale=-1.0), r=[eeb], w=[eeb])
                        dve(lambda e: e.tensor_tensor(out=zT[:, c2, cols], in0=dd[:], in1=ee[:], op=ALU.mult), r=[ddb, eeb], w=[zT_b[ch]])
                k.barrier()
            if dbg:
                k.dma("sp", lambda e: e.dma_start(out=dbg_d["d_zT"].ap()[s], in_=zT[:]), reads=zT_b)
            if stage <= 2:
                k.barrier()
                continue
            with ExitStack() as sh:
                wq = [salloc(sh, "wq%d" % i, [128, 8, 384], BF16) for i in range(2)]
                wq_b = [[Buf("wq%d_%d" % (i, j)) for j in range(3)] for i in range(2)]
                qT = salloc(sh, "qT", [128, S], BF16)
                kT = salloc(sh, "kT", [128, S], BF16)
                vT = salloc(sh, "vT", [128, S], BF16)
                qT_b = [Buf("qT%d" % i) for i in range(NCH)]
                kT_b = [Buf("kT%d" % i) for i in range(NCH)]
                vT_b = [Buf("vT%d" % i) for i in range(NCH)]
                acc = [salloc(sh, "acc%d" % i, [128, S], F32) for i in range(2)]
                acc_b = [Buf("acc%d" % i) for i in range(2)]
                qraw = [salloc(sh, "qraw%d" % i, [128, 512], BF16) for i in range(2)]
                qraw_b = [Buf("qraw%d" % i) for i in range(2)]
                t1 = [salloc(sh, "t1_%d" % i, [128, 512], F32) for i in range(2)]
                t1_b = [Buf("t1_%d" % i) for i in range(2)]
                t2 = [salloc(sh, "t2_%d" % i, [128, 512], F32) for i in range(2)]
                t2_b = [Buf("t2_%d" % i) for i in range(2)]
                RP = 4
                Pt = [salloc(sh, "Pt%d" % i, [128, 2, 256], BF16) for i in range(RP)]
                Pt_b = [Buf("Pt%d" % i) for i in range(RP)]
                vaug = [salloc(sh, "vaug%d" % i, [128, 2, 66], BF16) for i in range(RP)]
                vaug_b = [Buf("vaug%d" % i) for i in range(RP)]
                rden = [salloc(sh, "rden%d" % i, [128, 512], F32) for i in range(2)]
                rden_b = [Buf("rden%d" % i) for i in range(2)]
                oTc = [salloc(sh, "oTc%d" % i, [128, 512], BF16) for i in range(4)]
                oTc_b = [Buf("oTc%d" % i) for i in range(4)]
                for i in range(RP):
                    dve(lambda e: e.memset(vaug[i][:], 1.0), w=[vaug_b[i]])
                OBK = (5, 6)
                ob_b = [psb[5], psb[6]]
                BV = 4
                rq = 0
                for p in range(lim.get('pairs', 6)):
                    wb = p % 2
                    for j, c0 in enumerate((512 + 128 * p, 512 + 768 + 128 * p, 512 + 1536 + 128 * p)):
                        k.dma("pool", lambda e: e.dma_start(out=wq[wb][:, :, j * 128:(j + 1) * 128], in_=wslice(win, c0, 128)), writes=[wq_b[wb][j]])
                    for ch in range(NCH if lim.get('proj', 1) else 0):
                        cols = slice(ch * 512, (ch + 1) * 512)
                        banks = (5, 6, 7)
                        for j in range(3):
                            for c in range(8):
                                pe(lambda e: e.matmul(ps[banks[j]][:], lhsT=wq[wb][:, c, j * 128:(j + 1) * 128], rhs=hT[:, c, cols], start=(c == 0), stop=(c == 7)),
                                   r=[wq_b[wb][j], hT_b[ch]], w=[psb[banks[j]]], inc=(c == 7))
                        for j in range(2):
                            act(lambda e: e.activation(out=qraw[j][:], in_=ps[banks[j]][:], func=AF.Copy), r=[psb[banks[j]]], w=[qraw_b[j]])
                        act(lambda e: e.activation(out=vT[:, cols], in_=ps[banks[2]][:], func=AF.Copy), r=[psb[banks[2]]], w=[vT_b[ch]])
                        for j, (dst, dst_b) in enumerate(((qT, qT_b), (kT, kT_b))):
                            bank2 = (4, 3)[j]
                            pe(lambda e: e.matmul(ps[bank2][:], lhsT=rot_b[:], rhs=qraw[j][:], start=True, stop=True),
                               r=[rot_b_b, qraw_b[j]], w=[psb[bank2]])
                            dve(lambda e: e.tensor_tensor(out=t1[j][:], in0=ps[banks[j]][:], in1=cos_t[:, cols], op=ALU.mult), r=[psb[banks[j]], tab_b], w=[t1_b[j]])
                            dve(lambda e: e.tensor_tensor(out=t2[j][:], in0=ps[bank2][:], in1=sin_t[:, cols], op=ALU.mult), r=[psb[bank2], tab_b], w=[t2_b[j]])
                            dve(lambda e: e.tensor_tensor(out=dst[:, cols], in0=t1[j][:], in1=t2[j][:], op=ALU.add), r=[t1_b[j], t2_b[j]], w=[dst_b[ch]])
                    tiles = []
                    for bi, dil in enumerate((1, 4, 16)[:lim.get('branches', 3)]):
                        L = S // dil
                        NB = L // 128
                        for r in range(dil):
                            for j in range(NB + 1):
                                tiles.append((bi, dil, NB, r, j))

                    def a_front(ti):
                        bi, dil, NB, r, j = tiles[ti]
                        edge_f, edge_l = (j == 0), (j == NB)
                        nk = 64 if (edge_f or edge_l) else 128
                        ak0 = 0 if edge_f else 128 * j - 64
                        blocks = [b for b in (j - 1, j) if 0 <= b < NB]
                        aq0 = 128 * blocks[0]
                        nq = 128 * len(blocks)
                        ksl = slice(r + dil * ak0, r + dil * (ak0 + nk - 1) + 1, dil)
                        qsl = slice(r + dil * aq0, r + dil * (aq0 + nq - 1) + 1, dil)
                        tk0, tk1 = r + dil * ak0, r + dil * (ak0 + nk - 1)
                        tq0, tq1 = r + dil * aq0, r + dil * (aq0 + nq - 1)
                        kdeps = [kT_b[c] for c in range(tk0 // 512, tk1 // 512 + 1)]
                        vdeps = [vT_b[c] for c in range(tk0 // 512, tk1 // 512 + 1)]
                        qdeps = [qT_b[c] for c in range(tq0 // 512, tq1 // 512 + 1)]
                        vi = ti % RP
                        sdb = ti % 2
                        pe(lambda e: e.transpose(psbf[BV][0:nk, 0:128], vT[:, ksl], ident_b[:]), r=vdeps + [ident_b_b], w=[psb[BV]])
                        act(lambda e: e.activation(out=vaug[vi][0:nk, :, 0:64], in_=psbf[BV][0:nk, 0:128].rearrange("p (h d) -> p h d", h=2), func=AF.Copy),
                            r=[psb[BV]], w=[vaug_b[vi]])
                        for h in range(2):
                            pe(lambda e: e.matmul(ps[2 * sdb + h][0:nk, 0:nq], lhsT=kT[64 * h:64 * h + 64, ksl], rhs=qT[64 * h:64 * h + 64, qsl], start=True, stop=True),
                               r=kdeps + qdeps, w=[psb[2 * sdb + h]], inc=(h == 1))
                        act(lambda e: e.activation(out=Pt[vi][0:nk, :, 0:nq], in_=ps2[sdb][0:nk, :].rearrange("p (h q) -> p h q", h=2)[:, :, 0:nq], func=AF.Exp, scale=0.125),
                            r=[psb[2 * sdb], psb[2 * sdb + 1]], w=[Pt_b[vi]])
                        if edge_f:
                            mk, mkb = maskf[0:64, 0:128], maskf_b
                        elif edge_l:
                            mk, mkb = masklh[0:64, 0:128], masklh_b
                        else:
                            mk, mkb = masklh[:, 0:256], masklh_b
                        dve(lambda e: e.tensor_tensor(out=Pt[vi][0:nk, :, 0:nq], in0=Pt[vi][0:nk, :, 0:nq], in1=mk.unsqueeze(1).to_broadcast([nk, 2, nq]), op=ALU.mult),
                            r=[Pt_b[vi], mkb], w=[Pt_b[vi]])

                    def a_back(ti):
                        bi, dil, NB, r, j = tiles[ti]
                        nk = 64 if (j == 0 or j == NB) else 128
                        blocks = [b for b in (j - 1, j) if 0 <= b < NB]
                        vi = ti % RP
                        for bi2, b in enumerate(blocks):
                            for h in range(2):
                                oc0 = h * 128
                                pe(lambda e: e.matmul(ps[OBK[b % 2]][0:65, oc0:oc0 + 128], lhsT=vaug[vi][0:nk, h, 0:65], rhs=Pt[vi][0:nk, h, bi2 * 128:(bi2 + 1) * 128],
                                                      start=(j == b and h == 0), stop=(j == b + 1), skip_group_check=True),
                                   r=[vaug_b[vi], Pt_b[vi]], w=[ob_b[b % 2]], inc=(h == 1))
                        if j >= 1:
                            b = j - 1
                            tsl = slice(r + dil * 128 * b, r + dil * (128 * b + 127) + 1, dil)
                            for h in range(2):
                                oc0 = h * 128
                                if bi == 0:
                                    dve(lambda e: e.tensor_copy(out=acc[h][0:65, tsl], in_=ps[OBK[b % 2]][0:65, oc0:oc0 + 128]), r=[ob_b[b % 2]], w=[acc_b[h]])
                                else:
                                    dve(lambda e: e.tensor_tensor(out=acc[h][0:65, tsl], in0=acc[h][0:65, tsl], in1=ps[OBK[b % 2]][0:65, oc0:oc0 + 128], op=ALU.add),
                                        r=[ob_b[b % 2], acc_b[h]], w=[acc_b[h]])

                    PFA = 2
                    for ti in range(min(PFA, len(tiles))):
                        a_front(ti)
                    for ti in range(len(tiles)):
                        if ti + PFA < len(tiles):
                            a_front(ti + PFA)
                        a_back(ti)
                    for ch in range(NCH if lim.get('norm', 1) else 0):
                        cols = slice(ch * 512, (ch + 1) * 512)
                        for h in range(2):
                            i2 = (ch * 2 + h) % 2
                            i4 = (ch * 2 + h) % 4
                            bank = (7, 3)[i2]
                            pe(lambda e: e.matmul(ps[bank][0:64, :], lhsT=esel[0:65, 0:64], rhs=acc[h][0:65, cols], start=True, stop=True),
                               r=[esel_b, acc_b[h]], w=[psb[bank]])
                            act(lambda e: e.activation(out=rden[i2][0:64, :], in_=ps[bank][0:64, :], func=AF.Ln), r=[psb[bank]], w=[rden_b[i2]])
                            act(lambda e: e.activation(out=rden[i2][0:64, :], in_=rden[i2][0:64, :], func=AF.Exp, scale=-1.0), r=[rden_b[i2]], w=[rden_b[i2]])
                            dve(lambda e: e.tensor_tensor(out=oTc[i4][0:64, :], in0=acc[h][0:64, cols], in1=rden[i2][0:64, :], op=ALU.mult),
                                r=[acc_b[h], rden_b[i2]], w=[oTc_b[i4]])
                            row0 = p * 128 + h * 64
                            k.dma("sp", lambda e: e.dma_start(out=o_scr.ap()[s, row0:row0 + 64, cols], in_=oTc[i4][0:64, :]), reads=[oTc_b[i4]])
                k.barrier()
        if stage <= 3:
            k.barrier()
            continue
        k.join_detached()
        with ExitStack() as a2:
            wo = salloc(a2, "wo", [128, 8, D], BF16)
            wxq = salloc(a2, "wxq", [128, 8, D], BF16)
            wxo = salloc(a2, "wxo", [128, 8, D], BF16)
            wo_b, wxq_b, wxo_b = Buf("wo"), Buf("wxq"), Buf("wxo")
            KTm = salloc(a2, "KTm", [128, 8, MEM], BF16)
            Vm = salloc(a2, "Vm", [128, 2, D], BF16)
            KTm_b, Vm_b = Buf("KTm"), Buf("Vm")

            def wfull(name):
                return din[name].ap().rearrange("(c p) n -> p c n", p=128)

            k.dma("pool", lambda e: e.dma_start(out=wo[:], in_=wfull("w_out")), writes=[wo_b])
            with ExitStack() as am:
                wxk = salloc(am, "wxk", [128, 8, D], BF16)
                wxv = salloc(am, "wxv", [128, 8, D], BF16)
                wxk_b, wxv_b = Buf("wxk"), Buf("wxv")
                k.dma("pool", lambda e: e.dma_start(out=wxk[:], in_=wfull("w_xk")), writes=[wxk_b])
                k.dma("pool", lambda e: e.dma_start(out=wxv[:], in_=wfull("w_xv")), writes=[wxv_b])
                k.dma("pool", lambda e: e.dma_start(out=wxq[:], in_=wfull("w_xq")), writes=[wxq_b])
                k.dma("pool", lambda e: e.dma_start(out=wxo[:], in_=wfull("w_xo")), writes=[wxo_b])
                mt = [salloc(am, "mt%d" % i, [128, D], F32) for i in range(2)]
                mt_b = [Buf("mt%d" % i) for i in range(2)]
                mb = [salloc(am, "mb%d" % i, [128, D], BF16) for i in range(2)]
                mb_b = [Buf("mb%d" % i) for i in range(2)]
                mst = [salloc(am, "mst%d" % i, [128, 4], F32) for i in range(2)]
                mst_b = [Buf("mst%d" % i) for i in range(2)]
                memT = salloc(am, "memT", [128, 8, MEM], BF16)
                memT_b = Buf("memT")
                for m in range(2):
                    r0 = s * MEM + m * 128
                    k.dma("sp", lambda e: e.dma_start(out=mt[m][:], in_=din["mem"].ap()[r0:r0 + 128, :]), writes=[mt_b[m]])
                    act(lambda e: e.activation(out=mb[m][:], in_=mt[m][:], func=AF.Square, accum_out=mst[m][:, 0:1]), r=[mt_b[m]], w=[mb_b[m], mst_b[m]])
                    rstd_ops(mst[m], mst_b[m], 1.0 / D, EPS)
                    act(lambda e: e.activation(out=mb[m][:], in_=mt[m][:], func=AF.Copy, scale=mst[m][:, 2:3]), r=[mt_b[m], mst_b[m]], w=[mb_b[m]])
                    for c in range(8):
                        pe(lambda e: e.transpose(psbf[m][:, c * 128:(c + 1) * 128], mb[m][:, c * 128:(c + 1) * 128], ident_b[:]),
                           r=[mb_b[m], ident_b_b], w=[psb[m]], inc=(c == 7))
                    dve(lambda e: e.tensor_tensor(out=memT[:, :, m * 128:(m + 1) * 128], in0=psbf[m][:, :].rearrange("p (c t) -> p c t", c=8),
                                                  in1=g_mem[:, :].unsqueeze(2).to_broadcast([128, 8, 128]), op=ALU.mult),
                        r=[psb[m], g_mem_b], w=[memT_b])
                for oc in range(8):
                    bank = 2 + oc % 4
                    for c in range(8):
                        pe(lambda e: e.matmul(ps[bank][:, 0:MEM], lhsT=wxk[:, c, oc * 128:(oc + 1) * 128], rhs=memT[:, c, :], start=(c == 0), stop=(c == 7)),
                           r=[wxk_b, memT_b], w=[psb[bank]], inc=(c == 7))
                    act(lambda e: e.activation(out=KTm[:, oc, :], in_=ps[bank][:, 0:MEM], func=AF.Copy), r=[psb[bank]], w=[KTm_b])
                for m in range(2):
                    for half in range(2):
                        bank = 2 + (m * 2 + half)
                        for c in range(8):
                            pe(lambda e: e.matmul(ps[bank][:], lhsT=memT[:, c, m * 128:(m + 1) * 128], rhs=wxv[:, c, half * 512:(half + 1) * 512], start=(c == 0), stop=(c == 7)),
                               r=[wxv_b, memT_b], w=[psb[bank]], inc=(c == 7))
                        act(lambda e: e.activation(out=Vm[:, m, half * 512:(half + 1) * 512], in_=ps[bank][:], func=AF.Copy), r=[psb[bank]], w=[Vm_b])
                k.barrier()

            lgall = salloc(a2, "lgall", [128, NT, 36], F32)
            lgall_b = Buf("lgall")
            a2t = ExitStack()

            def ring(name, shape, dtype, n=2):
                return ([salloc(a2t, "%s%d" % (name, i), shape, dtype) for i in range(n)], [Buf("%s%d" % (name, i)) for i in range(n)])

            xt2, xt2_b = ring("xt2", [128, D], F32, 4)
            oTt, oTt_b = ring("oTt", [128, 6, 128], BF16, 4)
            sqb, sqb_b = ring("sqb", [128, 8, 128], BF16, 3)
            lnb, lnb_b = ring("lnb", [128, 2, 128], F32, 3)
            rsb, rsb_b = ring("rsb", [128, 2, 128], F32, 3)
            mixg, mixg_b = ring("mixg", [128, 8, 128], BF16, 3)
            st2, st2_b = ring("st2", [128, 8], F32, 3)
            x1, x1_b = ring("x1", [128, D], F32, 3)
            h2b, h2b_b = ring("h2b", [128, D], BF16, 3)
            h2T, h2T_b = ring("h2T", [128, 8, 128], BF16, 3)
            qxT, qxT_b = ring("qxT", [128, 8, 128], BF16, 3)
            Pm, Pm_b = ring("Pm", [128, 2, 4, 128], BF16, 3)
            rdx, rdx_b = ring("rdx", [128, 4, 128], F32, 3)
            oxT, oxT_b = ring("oxT", [128, 8, 128], BF16, 3)
            x2, x2_b = x1, x1_b
            h3b, h3b_b = ring("h3b", [128, D], BF16, 4)
            h3T, h3T_b = ring("h3T", [128, 8, 128], BF16, 3)

            def t_load(t):
                i4 = t % 4
                r0 = s * S + t * 128
                tok = slice(t * 128, (t + 1) * 128)
                k.dma("sp", lambda e: e.dma_start(out=xt2[i4][:], in_=x_d[r0:r0 + 128, :]), writes=[xt2_b[i4]])
                k.dma("sp", lambda e: e.dma_start(out=oTt[i4][:], in_=o_scr.ap()[s, :, tok].rearrange("(c p) t -> p c t", p=128)), writes=[oTt_b[i4]])

            def tile_gen(t):
                i = t % 3
                i4 = t % 4
                Ba, Bb = 2 * i, 2 * i + 1
                BK = (Ba, Bb)
                tt = s * NT + t
                r0 = s * S + t * 128
                tok = slice(t * 128, (t + 1) * 128)
                zb = zT_b[t // 4]
                act(lambda e: e.activation(out=sqb[i][:, 0:2, :], in_=zT[:, :, tok], func=AF.Square), r=[zb], w=[sqb_b[i]])
                act(lambda e: e.activation(out=sqb[i][:, 2:8, :], in_=oTt[i4][:], func=AF.Square), r=[oTt_b[i4]], w=[sqb_b[i]])
                yield
                for c in range(8):
                    col = 0 if c < 2 else 128
                    pe(lambda e: e.matmul(ps[Ba][:, col:col + 128], lhsT=ones_b[:], rhs=sqb[i][:, c, :], start=(c == 0), stop=(c == 1 or c == 7), skip_group_check=True),
                       r=[sqb_b[i], ones_b_b], w=[psb[Ba]], inc=(c == 7))
                yield
                act(lambda e: e.activation(out=lnb[i][:, 0, :], in_=ps[Ba][:, 0:128], func=AF.Ln, scale=1.0 / 256, bias=eps_ap(EPS)), r=[psb[Ba], epsb], w=[lnb_b[i]])
                act(lambda e: e.activation(out=lnb[i][:, 1, :], in_=ps[Ba][:, 128:256], func=AF.Ln, scale=1.0 / 768, bias=eps_ap(EPS)), r=[psb[Ba], epsb], w=[lnb_b[i]])
                act(lambda e: e.activation(out=rsb[i][:], in_=lnb[i][:], func=AF.Exp, scale=-0.5), r=[lnb_b[i]], w=[rsb_b[i]])
                yield
                for c in range(8):
                    src = zT[:, c, tok] if c < 2 else oTt[i4][:, c - 2, :]
                    k.op("dve", lambda e: e.scalar_tensor_tensor(out=mixg[i][:, c, :], in0=src, scalar=g_mx[:, c:c + 1], in1=rsb[i][:, 0 if c < 2 else 1, :], op0=ALU.mult, op1=ALU.mult),
                         [zb, oTt_b[i4], g_mx_b, rsb_b[i]], [mixg_b[i]], inc=(c == 7))
                yield
                for half in range(2):
                    hs = slice(half * 512, (half + 1) * 512)
                    for c in range(8):
                        pe(lambda e: e.matmul(ps[BK[half]][:], lhsT=mixg[i][:, c, :], rhs=wo[:, c, hs], start=(c == 0), stop=(c == 7)),
                           r=[mixg_b[i], wo_b], w=[psb[BK[half]]], inc=(c == 7))
                yield
                for half in range(2):
                    hs = slice(half * 512, (half + 1) * 512)
                    k.op("dve", lambda e: e.tensor_tensor(out=x1[i][:, hs], in0=ps[BK[half]][:], in1=xt2[i4][:, hs], op=ALU.add),
                         [psb[BK[half]], xt2_b[i4]], [x1_b[i]], inc=(half == 1))
                yield
                act(lambda e: e.activation(out=h2b[i][:], in_=x1[i][:], func=AF.Square, accum_out=st2[i][:, 4:5]), r=[x1_b[i]], w=[h2b_b[i], st2_b[i]])
                act(lambda e: e.activation(out=st2[i][:, 5:6], in_=st2[i][:, 4:5], func=AF.Ln, scale=1.0 / D, bias=eps_ap(EPS)), r=[st2_b[i], epsb], w=[st2_b[i]])
                yield
                act(lambda e: e.activation(out=st2[i][:, 6:7], in_=st2[i][:, 5:6], func=AF.Exp, scale=-0.5), r=[st2_b[i]], w=[st2_b[i]])
                act(lambda e: e.activation(out=h2b[i][:], in_=x1[i][:], func=AF.Copy, scale=st2[i][:, 6:7]), r=[x1_b[i], st2_b[i]], w=[h2b_b[i]])
                yield
                for c in range(8):
                    pe(lambda e: e.transpose(psbf[Ba][:, c * 128:(c + 1) * 128], h2b[i][:, c * 128:(c + 1) * 128], ident_b[:]),
                       r=[h2b_b[i], ident_b_b], w=[psb[Ba]], inc=(c == 7))
                yield
                dve(lambda e: e.tensor_tensor(out=h2T[i][:], in0=psbf[Ba][:, :].rearrange("p (c t) -> p c t", c=8),
                                              in1=g_xn[:, :].unsqueeze(2).to_broadcast([128, 8, 128]), op=ALU.mult),
                    r=[psb[Ba], g_xn_b], w=[h2T_b[i]])
                yield
                for hb_ in range(2):
                    bank = BK[hb_]
                    for o_ in range(4):
                        oc = hb_ * 4 + o_
                        for c in range(8):
                            pe(lambda e: e.matmul(ps[bank][:, o_ * 128:(o_ + 1) * 128], lhsT=wxq[:, c, oc * 128:(oc + 1) * 128], rhs=h2T[i][:, c, :], start=(c == 0), stop=(c == 7)),
                               r=[wxq_b, h2T_b[i]], w=[psb[bank]], inc=(c == 7 and o_ == 3))
                    yield
                for hb_ in range(2):
                    k.op("act", lambda e: e.activation(out=qxT[i][:, hb_ * 4:(hb_ + 1) * 4, :], in_=ps[BK[hb_]][:].rearrange("p (c t) -> p c t", c=4), func=AF.Copy),
                         [psb[BK[hb_]]], [qxT_b[i]], inc=True)
                yield
                for m in range(2):
                    for hd in range(4):
                        for c in range(2):
                            pe(lambda e: e.matmul(ps[BK[m]][:, hd * 128:(hd + 1) * 128], lhsT=KTm[:, hd * 2 + c, m * 128:(m + 1) * 128], rhs=qxT[i][:, hd * 2 + c, :],
                                                  start=(c == 0), stop=(c == 1)),
                               r=[KTm_b, qxT_b[i]], w=[psb[BK[m]]], inc=(c == 1 and hd == 3))
                yield
                for m in range(2):
                    act(lambda e: e.activation(out=Pm[i][:, m, :, :], in_=ps[BK[m]][:].rearrange("p (h t) -> p h t", h=4), func=AF.Exp, scale=1.0 / 16),
                        r=[psb[BK[m]]], w=[Pm_b[i]])
                yield
                for m in range(2):
                    pe(lambda e: e.matmul(ps[Ba][:], lhsT=ones_b[:], rhs=Pm[i][:, m, :, :], start=(m == 0), stop=(m == 1)),
                       r=[ones_b_b, Pm_b[i]], w=[psb[Ba]], inc=(m == 1))
                yield
                act(lambda e: e.activation(out=rdx[i][:], in_=ps[Ba][:].rearrange("p (h t) -> p h t", h=4), func=AF.Ln), r=[psb[Ba]], w=[rdx_b[i]])
                yield
                act(lambda e: e.activation(out=rdx[i][:], in_=rdx[i][:], func=AF.Exp, scale=-1.0), r=[rdx_b[i]], w=[rdx_b[i]])
                yield
                for hb_ in range(2):
                    bank = BK[hb_]
                    for o_ in range(4):
                        oc = hb_ * 4 + o_
                        hd = oc // 2
                        for m in range(2):
                            pe(lambda e: e.matmul(ps[bank][:, o_ * 128:(o_ + 1) * 128], lhsT=Vm[:, m, oc * 128:(oc + 1) * 128], rhs=Pm[i][:, m, hd, :], start=(m == 0), stop=(m == 1)),
                               r=[Vm_b, Pm_b[i]], w=[psb[bank]], inc=(m == 1 and o_ == 3))
                yield
                yield
                for hb_ in range(2):
                    k.op("dve", lambda e: e.tensor_tensor(out=oxT[i][:, hb_ * 4:(hb_ + 1) * 4, :].rearrange("p (h c) t -> p h c t", h=2),
                                                         in0=ps[BK[hb_]][:].rearrange("p (h c t) -> p h c t", h=2, c=2),
                                                         in1=rdx[i][:, hb_ * 2:(hb_ + 1) * 2, :].unsqueeze(2).to_broadcast([128, 2, 2, 128]), op=ALU.mult),
                         [psb[BK[hb_]], rdx_b[i]], [oxT_b[i]], inc=True)
                yield
                for half in range(2):
                    hs = slice(half * 512, (half + 1) * 512)
                    for c in range(8):
                        pe(lambda e: e.matmul(ps[BK[half]][:], lhsT=oxT[i][:, c, :], rhs=wxo[:, c, hs], start=(c == 0), stop=(c == 7)),
                           r=[oxT_b[i], wxo_b], w=[psb[BK[half]]], inc=(c == 7))
                yield
                for half in range(2):
                    hs = slice(half * 512, (half + 1) * 512)
                    k.op("dve", lambda e: e.tensor_tensor(out=x2[i][:, hs], in0=ps[BK[half]][:], in1=x1[i][:, hs], op=ALU.add),
                         [psb[BK[half]], x1_b[i]], [x2_b[i]], inc=(half == 1))
                k.dma("sp", lambda e: e.dma_start(out=x2_scr.ap()[r0:r0 + 128, :], in_=x2[i][:]), reads=[x2_b[i]])
                yield
                act(lambda e: e.activation(out=h3b[i4][:], in_=x2[i][:], func=AF.Square, accum_out=st2[i][:, 4:5]), r=[x2_b[i]], w=[h3b_b[i4], st2_b[i]])
                act(lambda e: e.activation(out=st2[i][:, 5:6], in_=st2[i][:, 4:5], func=AF.Ln, scale=1.0 / D, bias=eps_ap(EPS)), r=[st2_b[i], epsb], w=[st2_b[i]])
                yield
                act(lambda e: e.activation(out=st2[i][:, 7:8], in_=st2[i][:, 5:6], func=AF.Exp, scale=-0.5), r=[st2_b[i]], w=[st2_b[i]])
                yield
                dve(lambda e: e.scalar_tensor_tensor(out=h3b[i4][:], in0=x2[i][:], scalar=st2[i][:, 7:8], in1=gmoe[:], op0=ALU.mult, op1=ALU.mult),
                    r=[x2_b[i], st2_b[i], gmoe_b], w=[h3b_b[i4]])
                yield
                for c in range(8):
                    pe(lambda e: e.transpose(psbf[Ba][:, c * 128:(c + 1) * 128], h3b[i4][:, c * 128:(c + 1) * 128], ident_b[:]),
                       r=[h3b_b[i4], ident_b_b], w=[psb[Ba]], inc=(c == 7))
                yield
                act(lambda e: e.activation(out=h3T[i][:], in_=psbf[Ba][:, :].rearrange("p (c t) -> p c t", c=8), func=AF.Copy), r=[psb[Ba]], w=[h3T_b[i]])
                yield
                for c in range(8):
                    pe(lambda e: e.matmul(ps[Bb][:, 0:36], lhsT=h3T[i][:, c, :], rhs=wrb[:, c, :], start=(c == 0), stop=(c == 7)),
                       r=[h3T_b[i], wrb_b], w=[psb[Bb]], inc=(c == 7))
                yield
                dve(lambda e: e.tensor_tensor(out=lgall[:, t, :], in0=ps[Bb][:, 0:36], in1=brt[:], op=ALU.add), r=[psb[Bb], brt_b], w=[lgall_b])
                k.dma("sp", lambda e: e.dma_start(out=h3_scr.ap()[r0:r0 + 128, :], in_=h3b[i4][:]), reads=[h3b_b[i4]])

            NL = 3
            for t in range(min(4, NT)):
                t_load(t)
            active = []
            nxt = 0
            while nxt < NT or active:
                while len(active) < NL and nxt < NT:
                    active.append([tile_gen(nxt), nxt])
                    nxt += 1
                for a_ in list(active):
                    try:
                        next(a_[0])
                    except StopIteration:
                        active.remove(a_)
                        if a_[1] + 4 < NT:
                            t_load(a_[1] + 4)
            k.barrier()
            a2t.close()

            with ExitStack() as rs:
                NTl = NT

                def rbuf(name, shape, dtype=F32):
                    return salloc(rs, name, shape, dtype), Buf(name)

                gmx, gmx_b = rbuf("gmx", [128, NTl])
                goh, goh_b = rbuf("goh", [128, NTl, 4])
                gsh, gsh_b = rbuf("gsh", [128, NTl, 4])
                gsm, gsm_b = rbuf("gsm", [128, NTl])
                pgv, pgv_b = rbuf("pgv", [128, NTl])
                tmp4, tmp4_b = rbuf("tmp4", [128, NTl, 32])
                es8, es8_b = rbuf("es8", [128, NTl, 8])
                m1v, m1v_b = rbuf("m1v", [128, NTl])
                m2v, m2v_b = rbuf("m2v", [128, NTl])
                oh1, oh1_b = rbuf("oh1", [128, NTl, 8])
                oh2, oh2_b = rbuf("oh2", [128, NTl, 8])
                el2, el2_b = rbuf("el2", [128, NTl, 8])
                dmv, dmv_b = rbuf("dmv", [128, NTl])
                exv, exv_b = rbuf("exv", [128, NTl])
                w1v, w1v_b = rbuf("w1v", [128, NTl])
                w2v, w2v_b = rbuf("w2v", [128, NTl])
                o32 = [rbuf("o32_%d" % q, [128, NTl, 32]) for q in range(2)]
                selA, selA_b = rbuf("selA", [128, NTl, 32], BF16)
                rnk, rnk_b = rbuf("rnk", [128, NTl, 32])
                cnts, cnts_b = rbuf("cnts", [128, NTl, 32])
                bcum, bcum_b = rbuf("bcum", [128, NTl, 32])
                okv, okv_b = rbuf("okv", [128, NTl, 32])
                slf, slf_b = rbuf("slf", [128, NTl, 2])
                sidxA, sidxA_b = rbuf("sidxA", [128, NTl * 2], I32)

                lg3 = lgall[:, :, :]
                gl = lgall[:, :, 0:4]
                el = lgall[:, :, 4:36]
                el4 = el.rearrange("p t (g e) -> p t g e", g=4)
                dve(lambda e: e.tensor_reduce(out=gmx[:], in_=gl, axis=AX.X, op=ALU.max), r=[lgall_b], w=[gmx_b])
                dve(lambda e: e.tensor_tensor(out=goh[:], in0=gl, in1=gmx[:].unsqueeze(2).to_broadcast([128, NTl, 4]), op=ALU.is_equal), r=[lgall_b, gmx_b], w=[goh_b])
                dve(lambda e: e.tensor_tensor(out=gsh[:], in0=gl, in1=gmx[:].unsqueeze(2).to_broadcast([128, NTl, 4]), op=ALU.subtract), r=[lgall_b, gmx_b], w=[gsh_b])
                act(lambda e: e.activation(out=gsh[:], in_=gsh[:], func=AF.Exp), r=[gsh_b], w=[gsh_b])
                dve(lambda e: e.tensor_reduce(out=gsm[:], in_=gsh[:], axis=AX.X, op=ALU.add), r=[gsh_b], w=[gsm_b])
                dve(lambda e: e.reciprocal(out=pgv[:], in_=gsm[:]), r=[gsm_b], w=[pgv_b])
                dve(lambda e: e.tensor_tensor(out=tmp4[:].rearrange("p t (g e) -> p t g e", g=4), in0=el4, in1=goh[:].unsqueeze(3).to_broadcast([128, NTl, 4, 8]), op=ALU.mult),
                    r=[lgall_b, goh_b], w=[tmp4_b])
                dve(lambda e: e.tensor_reduce(out=es8[:], in_=tmp4[:].rearrange("p t (g e) -> p t e g", g=4), axis=AX.X, op=ALU.add), r=[tmp4_b], w=[es8_b])
                dve(lambda e: e.tensor_reduce(out=m1v[:], in_=es8[:], axis=AX.X, op=ALU.max), r=[es8_b], w=[m1v_b])
                dve(lambda e: e.tensor_tensor(out=oh1[:], in0=es8[:], in1=m1v[:].unsqueeze(2).to_broadcast([128, NTl, 8]), op=ALU.is_equal), r=[es8_b, m1v_b], w=[oh1_b])
                dve(lambda e: e.scalar_tensor_tensor(out=el2[:], in0=oh1[:], scalar=-1e30, in1=es8[:], op0=ALU.mult, op1=ALU.add), r=[oh1_b, es8_b], w=[el2_b])
                dve(lambda e: e.tensor_reduce(out=m2v[:], in_=el2[:], axis=AX.X, op=ALU.max), r=[el2_b], w=[m2v_b])
                dve(lambda e: e.tensor_tensor(out=oh2[:], in0=el2[:], in1=m2v[:].unsqueeze(2).to_broadcast([128, NTl, 8]), op=ALU.is_equal), r=[el2_b, m2v_b], w=[oh2_b])
                dve(lambda e: e.tensor_tensor(out=dmv[:], in0=m2v[:], in1=m1v[:], op=ALU.subtract), r=[m1v_b, m2v_b], w=[dmv_b])
                act(lambda e: e.activation(out=exv[:], in_=dmv[:], func=AF.Exp), r=[dmv_b], w=[exv_b])
                dve(lambda e: e.tensor_scalar(out=w1v[:], in0=exv[:], scalar1=1.0, scalar2=None, op0=ALU.add), r=[exv_b], w=[w1v_b])
                dve(lambda e: e.reciprocal(out=w1v[:], in_=w1v[:]), r=[w1v_b], w=[w1v_b])
                dve(lambda e: e.tensor_tensor(out=w2v[:], in0=exv[:], in1=w1v[:], op=ALU.mult), r=[exv_b, w1v_b], w=[w2v_b])
                gview = gate_f[:, s * NT * 2:(s + 1) * NT * 2].rearrange("p (t q) -> p t q", q=2)
                sview = slot_i[:, s * NT * 2:(s + 1) * NT * 2].rearrange("p (t q) -> p t q", q=2)
                dve(lambda e: e.tensor_tensor(out=gview[:, :, 0], in0=w1v[:], in1=pgv[:], op=ALU.mult), r=[w1v_b, pgv_b], w=[rt_b])
                dve(lambda e: e.tensor_tensor(out=gview[:, :, 1], in0=w2v[:], in1=pgv[:], op=ALU.mult), r=[w2v_b, pgv_b], w=[rt_b])
                for q, (ohq, ohq_b) in enumerate(((oh1, oh1_b), (oh2, oh2_b))):
                    dve(lambda e: e.tensor_tensor(out=o32[q][0][:].rearrange("p t (g e) -> p t g e", g=4), in0=goh[:].unsqueeze(3).to_broadcast([128, NTl, 4, 8]),
                                                  in1=ohq[:].unsqueeze(2).to_broadcast([128, NTl, 4, 8]), op=ALU.mult), r=[goh_b, ohq_b], w=[o32[q][1]])
                dve(lambda e: e.tensor_tensor(out=selA[:], in0=o32[0][0][:], in1=o32[1][0][:], op=ALU.add), r=[o32[0][1], o32[1][1]], w=[selA_b])
                for t in range(NTl):
                    bank = t // 16
                    c0 = (t % 16) * 32
                    pe(lambda e: e.matmul(ps[bank][:, c0:c0 + 32], lhsT=tri_b[:], rhs=selA[:, t, :], start=True, stop=True), r=[tri_b_b, selA_b], w=[psb[bank]], inc=False)
                    pe(lambda e: e.matmul(ps[2 + bank][:, c0:c0 + 32], lhsT=ones_b[:], rhs=selA[:, t, :], start=True, stop=True), r=[ones_b_b, selA_b], w=[psb[2 + bank]], inc=(t % 16 == 15))
                for bank in range(2):
                    dve(lambda e: e.tensor_copy(out=rnk[:, bank * 16:(bank + 1) * 16, :], in_=ps[bank][:].rearrange("p (t e) -> p t e", e=32)), r=[psb[bank]], w=[rnk_b])
                    dve(lambda e: e.tensor_copy(out=cnts[:, bank * 16:(bank + 1) * 16, :], in_=ps[2 + bank][:].rearrange("p (t e) -> p t e", e=32)), r=[psb[2 + bank]], w=[cnts_b])
                dve(lambda e: e.tensor_copy(out=bcum[:, 0, :], in_=base_t[:]), r=[base_b], w=[bcum_b])
                for t in range(1, NTl):
                    dve(lambda e: e.tensor_tensor(out=bcum[:, t, :], in0=bcum[:, t - 1, :], in1=cnts[:, t - 1, :], op=ALU.add), r=[bcum_b, cnts_b], w=[bcum_b])
                dve(lambda e: e.tensor_tensor(out=base_t[:], in0=bcum[:, NTl - 1, :], in1=cnts[:, NTl - 1, :], op=ALU.add), r=[bcum_b, cnts_b], w=[base_b])
                dve(lambda e: e.tensor_tensor(out=rnk[:], in0=rnk[:], in1=bcum[:], op=ALU.add), r=[rnk_b, bcum_b], w=[rnk_b])
                dve(lambda e: e.tensor_scalar(out=okv[:], in0=rnk[:], scalar1=float(CAP), scalar2=None, op0=ALU.is_lt), r=[rnk_b], w=[okv_b])
                dve(lambda e: e.tensor_tensor(out=rnk[:], in0=rnk[:], in1=ec_t[:].unsqueeze(1).to_broadcast([128, NTl, 32]), op=ALU.add), r=[rnk_b, ec_b], w=[rnk_b])
                dve(lambda e: e.scalar_tensor_tensor(out=rnk[:], in0=rnk[:], scalar=float(-NSLOT), in1=okv[:], op0=ALU.add, op1=ALU.mult), r=[rnk_b, okv_b], w=[rnk_b])
                for q in range(2):
                    dve(lambda e: e.tensor_tensor(out=o32[q][0][:], in0=o32[q][0][:], in1=rnk[:], op=ALU.mult), r=[o32[q][1], rnk_b], w=[o32[q][1]])
                    dve(lambda e: e.tensor_reduce(out=slf[:, :, q], in_=o32[q][0][:], axis=AX.X, op=ALU.add), r=[o32[q][1]], w=[slf_b])
                dve(lambda e: e.tensor_scalar(out=slf[:], in0=slf[:], scalar1=float(NSLOT), scalar2=None, op0=ALU.add), r=[slf_b], w=[slf_b])
                dve(lambda e: e.tensor_copy(out=sview, in_=slf[:]), r=[slf_b], w=[rt_b])
                dve(lambda e: e.tensor_copy(out=sidxA[:].rearrange("p (t q) -> p t q", q=2), in_=slf[:]), r=[slf_b], w=[sidxA_b])
                k.barrier()
                for t in range(NTl):
                    pend_scatter.append((s, t))
            if s == nseq - 1:
                while scatter_step():
                    pass
            k.barrier()
    if dbg:
        k.dma("sp", lambda e: e.dma_start(out=dbg_d["d_rt"].ap()[:, 0:NTT * 2], in_=gate_f[:]), reads=[rt_b])
        k.barrier()
    if stage <= 4:
        k.finish("sp")
        return nc, dbg_d

    NST = CAP // 128
    k.join_detached()
    k.barrier()
    with ExitStack() as sb:
        w1b = [salloc(sb, "w1b%d" % i, [128, 8, 512], BF16) for i in range(2)]
        w3b = [salloc(sb, "w3b%d" % i, [128, 8, 512], BF16) for i in range(2)]
        w2b = [salloc(sb, "w2b%d" % i, [128, 4, D], BF16) for i in range(2)]
        w1_b = [Buf("w1b%d" % i) for i in range(2)]
        w3_b = [Buf("w3b%d" % i) for i in range(2)]
        w2_b = [Buf("w2b%d" % i) for i in range(2)]
        xrow = [salloc(sb, "xrow%d" % i, [128, D], BF16) for i in range(3)]
        xrow_b = [Buf("xrow%d" % i) for i in range(3)]
        xsT = [salloc(sb, "xsT%d" % i, [128, 8, CAP], BF16) for i in range(2)]
        xsT_b = [Buf("xsT%d" % i) for i in range(2)]
        HT = [salloc(sb, "HT%d" % i, [128, 4, CAP], BF16) for i in range(2)]
        HT_b = [Buf("HT%d" % i) for i in range(2)]
        tf = [salloc(sb, "etf%d" % i, [128, 512], F32) for i in range(3)]
        tf_b = [Buf("etf%d" % i) for i in range(3)]
        yt = [salloc(sb, "yt%d" % i, [128, D], BF16) for i in range(3)]
        yt_b = [Buf("yt%d" % i) for i in range(3)]

        def load_w(e_):
            wb = e_ % 2
            k.dma("pool", lambda e: e.dma_start(out=w1b[wb][:], in_=din["w1"].ap()[e_].rearrange("(c p) f -> p c f", p=128)), writes=[w1_b[wb]])
            k.dma("pool", lambda e: e.dma_start(out=w3b[wb][:], in_=din["w3"].ap()[e_].rearrange("(c p) f -> p c f", p=128)), writes=[w3_b[wb]])
            k.dma("pool", lambda e: e.dma_start(out=w2b[wb][:], in_=din["w2"].ap()[e_].rearrange("(c p) d -> p c d", p=128)), writes=[w2_b[wb]])

        cregs = [None, None]
        load_w(0)
        cnt_ = {"rx": 0, "ry": 0, "rt3": 0}
        NE = NEXP if stage > 5 or stage == 99 else lim.get("nexp", NEXP)

        def b_front(e_):
            wb = e_ % 2
            for st_ in range(NST):
                xi = cnt_["rx"] % 3
                cnt_["rx"] += 1
                r0 = e_ * CAP + st_ * 128
                k.dma("sp", lambda e: e.dma_start(out=xrow[xi][:], in_=xs_scr.ap()[r0:r0 + 128, :]), writes=[xrow_b[xi]])
                bank = 6 + st_ % 2

                def body():
                    for c in range(8):
                        pe(lambda e: e.transpose(psbf[bank][:, c * 128:(c + 1) * 128], xrow[xi][:, c * 128:(c + 1) * 128], ident_b[:]),
                           r=[xrow_b[xi], ident_b_b], w=[psb[bank]], inc=(c == 7))
                body()
                act(lambda e: e.activation(out=xsT[wb][:, :, st_ * 128:(st_ + 1) * 128], in_=psbf[bank][:, :].rearrange("p (c t) -> p c t", c=8), func=AF.Copy),
                    r=[psb[bank]], w=[xsT_b[wb]])

        def b_stage1(e_):
            wb = e_ % 2
            for (s0, n) in ((0, 512), (512, CAP - 512)):
                for f in range(4):
                    b1, b3 = (0, 1) if f % 2 == 0 else (2, 3)

                    def body():
                        for c in range(8):
                            pe(lambda e: e.matmul(ps[b1][:, 0:n], lhsT=w1b[wb][:, c, f * 128:(f + 1) * 128], rhs=xsT[wb][:, c, s0:s0 + n], start=(c == 0), stop=(c == 7)),
                               r=[w1_b[wb], xsT_b[wb]], w=[psb[b1]], inc=(c == 7))
                        for c in range(8):
                            pe(lambda e: e.matmul(ps[b3][:, 0:n], lhsT=w3b[wb][:, c, f * 128:(f + 1) * 128], rhs=xsT[wb][:, c, s0:s0 + n], start=(c == 0), stop=(c == 7)),
                               r=[w3_b[wb], xsT_b[wb]], w=[psb[b3]], inc=(c == 7))
                    if s0 == 0:
                        body()
                    else:
                        k.pe_cond(cregs[wb], s0 + 1, 2, body)
                    ti = cnt_["rt3"] % 3
                    cnt_["rt3"] += 1
                    act(lambda e: e.activation(out=tf[ti][:, 0:n], in_=ps[b1][:, 0:n], func=AF.Tanh, scale=0.5), r=[psb[b1]], w=[tf_b[ti]])
                    dve(lambda e: e.scalar_tensor_tensor(out=tf[ti][:, 0:n], in0=tf[ti][:, 0:n], scalar=1.0, in1=ps[b1][:, 0:n], op0=ALU.add, op1=ALU.mult),
                        r=[psb[b1], tf_b[ti]], w=[tf_b[ti]])
                    dve(lambda e: e.tensor_tensor(out=HT[wb][:, f, s0:s0 + n], in0=ps[b3][:, 0:n], in1=tf[ti][:, 0:n], op=ALU.mult), r=[psb[b3], tf_b[ti]], w=[HT_b[wb]])

        def b_stage2(e_):
            wb = e_ % 2
            for st_ in range(NST):
                yi = cnt_["ry"] % 3
                cnt_["ry"] += 1
                for half in range(2):
                    bank = 4 + half

                    def body():
                        for f in range(4):
                            pe(lambda e: e.matmul(ps[bank][:], lhsT=HT[wb][:, f, st_ * 128:(st_ + 1) * 128], rhs=w2b[wb][:, f, half * 512:(half + 1) * 512], start=(f == 0), stop=(f == 3)),
                               r=[HT_b[wb], w2_b[wb]], w=[psb[bank]], inc=(f == 3))
                    if st_ == 0:
                        body()
                    else:
                        k.pe_cond(cregs[wb], st_ * 128 + 1, 1, body)
                    act(lambda e: e.activation(out=yt[yi][:, half * 512:(half + 1) * 512], in_=ps[bank][:], func=AF.Copy, scale=0.5), r=[psb[bank]], w=[yt_b[yi]])
                r0 = e_ * CAP + st_ * 128
                k.dma("pool", lambda e: e.dma_start(out=ys_scr.ap()[r0:r0 + 128, :], in_=yt[yi][:]), reads=[yt_b[yi]])

        b_front(0)
        for e_ in range(NE):
            if e_ + 1 < NEXP:
                load_w(e_ + 1)
            b_stage1(e_)
            if e_ + 1 < NE:
                b_front(e_ + 1)
            b_stage2(e_)
        k.barrier()

    with ExitStack() as sc_:
        RC = 4
        x2t = [salloc(sc_, "x2t%d" % i, [128, D], F32) for i in range(RC)]
        x2t_b = [Buf("x2t%d" % i) for i in range(RC)]
        yg = [[salloc(sc_, "yg%d_%d" % (i, q), [128, D], BF16) for q in range(2)] for i in range(RC)]
        yg_b = [[Buf("yg%d_%d" % (i, q)) for q in range(2)] for i in range(RC)]
        gi = [[salloc(sc_, "gi%d_%d" % (i, q), [128, 1], I32) for q in range(2)] for i in range(RC)]
        gi_b = [[Buf("gi%d_%d" % (i, q)) for q in range(2)] for i in range(RC)]
        x3 = [salloc(sc_, "x3_%d" % i, [128, D], F32) for i in range(2)]
        x3_b = [Buf("x3_%d" % i) for i in range(2)]
        jk = [salloc(sc_, "jk%d" % i, [128, D], BF16) for i in range(2)]
        jk_b = [Buf("jk%d" % i) for i in range(2)]
        st3 = [salloc(sc_, "st3_%d" % i, [128, 4], F32) for i in range(2)]
        st3_b = [Buf("st3_%d" % i) for i in range(2)]
        ot = [salloc(sc_, "ot%d" % i, [128, D], F32) for i in range(2)]
        ot_b = [Buf("ot%d" % i) for i in range(2)]

        def c_front(tt):
            i = tt % RC
            r0 = tt * 128
            k.dma("sp", lambda e: e.dma_start(out=x2t[i][:], in_=x2_scr.ap()[r0:r0 + 128, :]), writes=[x2t_b[i]])
            for q in range(2):
                dve(lambda e: e.tensor_copy(out=gi[i][q][:, :], in_=slot_i[:, 2 * tt + q:2 * tt + q + 1]), r=[rt_b], w=[gi_b[i][q]])
                k.dma("pool", lambda e: e.indirect_dma_start(out=yg[i][q][:, :], out_offset=None, in_=ys_scr.ap(),
                                                           in_offset=bass.IndirectOffsetOnAxis(ap=gi[i][q][:, :], axis=0)),
                      reads=[gi_b[i][q]], writes=[yg_b[i][q]], pre_nop=True)

        def c_back(tt):
            i = tt % RC
            j = tt % 2
            r0 = tt * 128
            dve(lambda e: e.scalar_tensor_tensor(out=x3[j][:], in0=yg[i][0][:], scalar=gate_f[:, 2 * tt:2 * tt + 1], in1=x2t[i][:], op0=ALU.mult, op1=ALU.add),
                r=[yg_b[i][0], rt_b, x2t_b[i]], w=[x3_b[j]])
            dve(lambda e: e.scalar_tensor_tensor(out=x3[j][:], in0=yg[i][1][:], scalar=gate_f[:, 2 * tt + 1:2 * tt + 2], in1=x3[j][:], op0=ALU.mult, op1=ALU.add),
                r=[yg_b[i][1], rt_b, x3_b[j]], w=[x3_b[j]])
            act(lambda e: e.activation(out=jk[j][:], in_=x3[j][:], func=AF.Square, accum_out=st3[j][:, 0:1]), r=[x3_b[j]], w=[jk_b[j], st3_b[j]])
            rstd_ops(st3[j], st3_b[j], 1.0 / D, EPS)
            dve(lambda e: e.scalar_tensor_tensor(out=ot[j][:], in0=x3[j][:], scalar=st3[j][:, 2:3], in1=gfin[:], op0=ALU.mult, op1=ALU.mult),
                r=[x3_b[j], st3_b[j], gfin_b], w=[ot_b[j]])
            k.dma("sp", lambda e: e.dma_start(out=out_d.ap()[r0:r0 + 128, :], in_=ot[j][:]), reads=[ot_b[j]])

        PFC = 2
        for tt in range(min(PFC, NTT)):
            c_front(tt)
        for tt in range(NTT):
            if tt + PFC < NTT:
                c_front(tt + PFC)
            c_back(tt)
        k.barrier()

    k.finish("sp")
    return nc, dbg_d


def make_in_map(inputs, core, nseq):
    m = {}
    b0 = core * nseq
    m["x"] = np.ascontiguousarray(np.asarray(inputs["x"])[b0:b0 + nseq].reshape(nseq * S, D))
    m["mem"] = np.ascontiguousarray(np.asarray(inputs["mem"])[b0:b0 + nseq].reshape(nseq * MEM, D))
    m["positions"] = np.ascontiguousarray(np.asarray(inputs["positions"]).astype(np.int32))
    for k_, shp in WEIGHT_SHAPES.items():
        a = np.asarray(inputs[k_])
        if k_ != "final_norm_g":
            a = a[0]
        m[k_] = np.ascontiguousarray(a.reshape(shp).astype(np.float32))
    m.update(host_consts())
    return m


_NC_CACHE = {}


def kernel(**inputs):
    n_cores = 8
    nseq = 2
    if "nc" not in _NC_CACHE:
        _NC_CACHE["nc"] = build(nseq=nseq)[0]
    nc = _NC_CACHE["nc"]
    consts = host_consts()
    shared = None
    in_maps = []
    for c in range(n_cores):
        m = make_in_map(inputs, c, nseq) if shared is None else dict(shared)
        if shared is None:
            shared = {k_: v for k_, v in m.items() if k_ not in ("x", "mem")}
        else:
            b0 = c * nseq
            m["x"] = np.ascontiguousarray(np.asarray(inputs["x"])[b0:b0 + nseq].reshape(nseq * S, D))
            m["mem"] = np.ascontiguousarray(np.asarray(inputs["mem"])[b0:b0 + nseq].reshape(nseq * MEM, D))
        in_maps.append(m)
    res = run_bass_kernel_spmd(nc, in_maps, core_ids=list(range(n_cores)))
    outs = [np.asarray(r["out"]).reshape(nseq, S, D) for r in res.results]
    return np.concatenate(outs, axis=0).astype(np.float32)
```
